# Optimizing a Trainium2 kernel written in Bass

```python
import math
import jax, jax.numpy as jnp
from jax import lax
import numpy as np

D_MODEL = 1024
BATCH = 8
SEQ = 8192
DEPTH = 2

HEAD_DIM = 64
Q_BLOCK = 128
RMS_EPS = 1e-6
FOX_HEADS = 8
DSA_HEADS = 8
IDX_HEADS = 4
IDX_DIM = 64
DSA_TOPK = 256
DIFF_HEADS = 4
DIFF_VDIM = 2 * HEAD_DIM
MLA_HEADS = 8
MLA_NOPE = 64
MLA_ROPE = 32
MLA_VDIM = 64
MLA_Q_RANK = 256
MLA_KV_RANK = 128
ROPE_THETA = 10000.0
T5_BUCKETS = 32
T5_MAX_DIST = 128
T5_HEADS = 8
FFN_HIDDEN = 256 * math.ceil(8 * D_MODEL / (3 * 256))

FOX_W = FOX_HEADS * HEAD_DIM
DSA_W = DSA_HEADS * HEAD_DIM
EVEN_SPLITS = [FOX_W, FOX_W, FOX_W, FOX_HEADS, DSA_W, DSA_W, DSA_W,
               IDX_HEADS * IDX_DIM, IDX_DIM, IDX_HEADS]
EVEN_IN = sum(EVEN_SPLITS)
EVEN_MIX = FOX_W + DSA_W
DIFF_QK_W = DIFF_HEADS * 2 * HEAD_DIM
DIFF_V_W = DIFF_HEADS * DIFF_VDIM
MLA_OUT_W = MLA_HEADS * MLA_VDIM
ODD_SPLITS = [DIFF_QK_W, DIFF_QK_W, DIFF_V_W, MLA_Q_RANK, MLA_KV_RANK, MLA_ROPE]
ODD_IN = sum(ODD_SPLITS)
ODD_MIX = DIFF_V_W + MLA_OUT_W
N_EVEN = (DEPTH + 1) // 2
N_ODD = DEPTH // 2

kernel_name = "hybrid_fox_dsa_diff_mla_block"

F32 = jnp.float32


def rms_norm(x, g):
    x32 = x.astype(F32)
    y = x32 * lax.rsqrt(jnp.mean(x32 * x32, axis=-1, keepdims=True) + RMS_EPS)
    return (y * g.astype(F32)).astype(x.dtype)


def split_cols(y, sizes):
    offs = [int(o) for o in np.cumsum(sizes)[:-1]]
    return jnp.split(y, offs, axis=-1)


def causal_mask(t_pos, s_len):
    return jnp.arange(s_len)[None, :] <= t_pos[:, None]


def masked_softmax(logits, mask):
    return jax.nn.softmax(jnp.where(mask, logits, -jnp.inf), axis=-1)


def t5_bucket(dist):
    exact = T5_BUCKETS // 2
    d = jnp.maximum(dist, 1).astype(F32)
    log_b = exact + (jnp.log(d / exact) / math.log(T5_MAX_DIST / exact)
                     * (T5_BUCKETS - exact)).astype(jnp.int32)
    log_b = jnp.minimum(log_b, T5_BUCKETS - 1)
    return jnp.where(dist < exact, dist, log_b)


def dense_t5_bias(t_pos, s_len, table):
    dist = jnp.maximum(t_pos[:, None] - jnp.arange(s_len)[None, :], 0)
    return jnp.transpose(table[t5_bucket(dist)], (2, 0, 1)).astype(F32)


def sweep_query_blocks(block_fn, seq_len):
    n = seq_len // Q_BLOCK
    out = lax.map(block_fn, jnp.arange(n))
    out = jnp.moveaxis(out, 0, 1)
    return out.reshape(out.shape[0], n * Q_BLOCK, *out.shape[3:])


def rope_tables(s_len):
    pos = jnp.arange(s_len, dtype=F32)
    inv = ROPE_THETA ** (-jnp.arange(0, MLA_ROPE, 2, dtype=F32) / MLA_ROPE)
    ang = pos[:, None] * inv[None, :]
    ang = jnp.concatenate([ang, ang], axis=-1)
    return jnp.cos(ang), jnp.sin(ang)


def apply_rope(x, cos, sin):
    x32 = x.astype(F32)
    x1, x2 = jnp.split(x32, 2, axis=-1)
    rot = jnp.concatenate([-x2, x1], axis=-1)
    return (x32 * cos + rot * sin).astype(x.dtype)


def fox_attention(q, k, v, log_cum):
    s_len = q.shape[1]
    scale = HEAD_DIM ** -0.5

    def block(i):
        start = i * Q_BLOCK
        t_pos = start + jnp.arange(Q_BLOCK)
        qb = lax.dynamic_slice_in_dim(q, start, Q_BLOCK, 1)
        cb = lax.dynamic_slice_in_dim(log_cum, start, Q_BLOCK, 2)
        logits = jnp.einsum('bqhd,bkhd->bhqk', qb, k, preferred_element_type=F32) * scale
        logits = logits + (cb[..., None] - log_cum[:, :, None, :])
        p = masked_softmax(logits, causal_mask(t_pos, s_len))
        return jnp.einsum('bhqk,bkhd->bqhd', p.astype(v.dtype), v)

    return sweep_query_blocks(block, s_len)


def dsa_attention(q, k, v, q_idx, k_idx, w_idx, t5_table):
    s_len = q.shape[1]
    k_sel = min(DSA_TOPK, s_len // 4)
    scale = HEAD_DIM ** -0.5
    idx_scale = (IDX_DIM ** -0.5) * (IDX_HEADS ** -0.5)
    gather = jax.vmap(lambda a, ix: a[ix])

    def block(i):
        start = i * Q_BLOCK
        t_pos = start + jnp.arange(Q_BLOCK)
        qb = lax.dynamic_slice_in_dim(q, start, Q_BLOCK, 1)
        qib = lax.dynamic_slice_in_dim(q_idx, start, Q_BLOCK, 1)
        wb = lax.dynamic_slice_in_dim(w_idx, start, Q_BLOCK, 1)
        rel = jax.nn.relu(jnp.einsum('bqhd,bkd->bqhk', qib, k_idx, preferred_element_type=F32))
        score = jnp.einsum('bqhk,bqh->bqk', rel, wb.astype(F32)) * idx_scale
        score = jnp.where(causal_mask(t_pos, s_len)[None], score, -jnp.inf)
        _, sel = lax.top_k(score, k_sel)
        valid = sel <= t_pos[None, :, None]
        ks = gather(k, sel)
        vs = gather(v, sel)
        logits = jnp.einsum('bqhd,bqkhd->bhqk', qb, ks, preferred_element_type=F32) * scale
        dist = jnp.maximum(t_pos[None, :, None] - sel, 0)
        bias = t5_table[t5_bucket(dist)]
        logits = logits + jnp.transpose(bias, (0, 3, 1, 2)).astype(F32)
        p = masked_softmax(logits, valid[:, None])
        return jnp.einsum('bhqk,bqkhd->bqhd', p.astype(vs.dtype), vs)

    return sweep_query_blocks(block, s_len)


def diff_attention(q, k, v, lam, t5_table):
    bsz, s_len = q.shape[:2]
    scale = HEAD_DIM ** -0.5

    def block(i):
        start = i * Q_BLOCK
        t_pos = start + jnp.arange(Q_BLOCK)
        qb = lax.dynamic_slice_in_dim(q, start, Q_BLOCK, 1)
        logits = jnp.einsum('bqhd,bkhd->bhqk', qb, k, preferred_element_type=F32) * scale
        logits = logits + dense_t5_bias(t_pos, s_len, t5_table)[None]
        p = masked_softmax(logits, causal_mask(t_pos, s_len))
        p = p.reshape(bsz, DIFF_HEADS, 2, Q_BLOCK, s_len)
        a = p[:, :, 0] - lam * p[:, :, 1]
        return jnp.einsum('bhqk,bkhd->bqhd', a.astype(v.dtype), v)

    return sweep_query_blocks(block, s_len)


def mla_attention(q_nope, q_rope, k_nope, k_rope, v):
    s_len = q_nope.shape[1]
    scale = (MLA_NOPE + MLA_ROPE) ** -0.5

    def block(i):
        start = i * Q_BLOCK
        t_pos = start + jnp.arange(Q_BLOCK)
        qn = lax.dynamic_slice_in_dim(q_nope, start, Q_BLOCK, 1)
        qr = lax.dynamic_slice_in_dim(q_rope, start, Q_BLOCK, 1)
        logits = (jnp.einsum('bqhd,bkhd->bhqk', qn, k_nope, preferred_element_type=F32)
                  + jnp.einsum('bqhr,bkr->bhqk', qr, k_rope, preferred_element_type=F32)) * scale
        p = masked_softmax(logits, causal_mask(t_pos, s_len))
        return jnp.einsum('bhqk,bkhd->bqhd', p.astype(v.dtype), v)

    return sweep_query_blocks(block, s_len)


def even_mixer(h, w_in, b_forget, w_out, t5_table):
    bsz, s_len, _ = h.shape
    y = h @ w_in
    fq, fk, fv, ff, dq, dk, dv, iq, ik, iw = split_cols(y, EVEN_SPLITS)
    hd = lambda a, n, d: a.reshape(bsz, s_len, n, d)
    log_f = jax.nn.log_sigmoid(ff.astype(F32) + b_forget.astype(F32))
    log_cum = jnp.transpose(jnp.cumsum(log_f, axis=1), (0, 2, 1))
    fox_out = fox_attention(hd(fq, FOX_HEADS, HEAD_DIM), hd(fk, FOX_HEADS, HEAD_DIM),
                            hd(fv, FOX_HEADS, HEAD_DIM), log_cum)
    dsa_out = dsa_attention(hd(dq, DSA_HEADS, HEAD_DIM), hd(dk, DSA_HEADS, HEAD_DIM),
                            hd(dv, DSA_HEADS, HEAD_DIM), hd(iq, IDX_HEADS, IDX_DIM),
                            ik, iw, t5_table)
    mixed = jnp.concatenate([fox_out.reshape(bsz, s_len, FOX_W),
                             dsa_out.reshape(bsz, s_len, DSA_W)], axis=-1)
    return mixed @ w_out


def odd_mixer(h, w_in, lq1, lk1, lq2, lk2, subln_g, q_norm_g, w_uq, kv_norm_g, w_ukv,
              w_out, t5_table, lambda_init):
    bsz, s_len, _ = h.shape
    y = h @ w_in
    cq, ck, cv, mcq, mckv, mkr = split_cols(y, ODD_SPLITS)
    lam = (jnp.exp(jnp.sum(lq1.astype(F32) * lk1.astype(F32)))
           - jnp.exp(jnp.sum(lq2.astype(F32) * lk2.astype(F32))) + lambda_init)
    diff_out = diff_attention(cq.reshape(bsz, s_len, 2 * DIFF_HEADS, HEAD_DIM),
                              ck.reshape(bsz, s_len, 2 * DIFF_HEADS, HEAD_DIM),
                              cv.reshape(bsz, s_len, DIFF_HEADS, DIFF_VDIM), lam, t5_table)
    diff_out = rms_norm(diff_out, subln_g) * (1.0 - lambda_init)
    q = (rms_norm(mcq, q_norm_g) @ w_uq).reshape(bsz, s_len, MLA_HEADS, MLA_NOPE + MLA_ROPE)
    q_nope, q_rope = q[..., :MLA_NOPE], q[..., MLA_NOPE:]
    kv = (rms_norm(mckv, kv_norm_g) @ w_ukv).reshape(bsz, s_len, MLA_HEADS, MLA_NOPE + MLA_VDIM)
    k_nope, v = kv[..., :MLA_NOPE], kv[..., MLA_NOPE:]
    cos, sin = rope_tables(s_len)
    q_rope = apply_rope(q_rope, cos[:, None, :], sin[:, None, :])
    k_rope = apply_rope(mkr, cos, sin)
    mla_out = mla_attention(q_nope, q_rope, k_nope, k_rope, v)
    mixed = jnp.concatenate([diff_out.reshape(bsz, s_len, DIFF_V_W),
                             mla_out.reshape(bsz, s_len, MLA_OUT_W)], axis=-1)
    return mixed @ w_out


def swiglu(h, w_gate, w_up, w_down):
    return (jax.nn.silu(h @ w_gate) * (h @ w_up)) @ w_down


def setup_inputs(seed: int = 0) -> dict:
    key = jax.random.key(seed)
    ks = jax.random.split(key, 24)
    nrm = lambda k, shape, fan_in: jax.random.normal(k, shape, F32) * (fan_in ** -0.5)
    gain = lambda k, shape: 1.0 + 0.02 * jax.random.normal(k, shape, F32)
    return {
        "x": jax.random.normal(ks[0], (BATCH, SEQ, D_MODEL), F32),
        "norm_mix_g": gain(ks[1], (DEPTH, D_MODEL)),
        "norm_ffn_g": gain(ks[2], (DEPTH, D_MODEL)),
        "w_in_even": nrm(ks[3], (N_EVEN, D_MODEL, EVEN_IN), D_MODEL),
        "b_forget": jax.random.uniform(ks[4], (N_EVEN, FOX_HEADS), F32, 1.0, 4.0),
        "w_out_even": nrm(ks[5], (N_EVEN, EVEN_MIX, D_MODEL), EVEN_MIX),
        "w_in_odd": nrm(ks[6], (N_ODD, D_MODEL, ODD_IN), D_MODEL),
        "lambda_q1": 0.1 * jax.random.normal(ks[7], (N_ODD, HEAD_DIM), F32),
        "lambda_k1": 0.1 * jax.random.normal(ks[8], (N_ODD, HEAD_DIM), F32),
        "lambda_q2": 0.1 * jax.random.normal(ks[9], (N_ODD, HEAD_DIM), F32),
        "lambda_k2": 0.1 * jax.random.normal(ks[10], (N_ODD, HEAD_DIM), F32),
        "diff_subln_g": gain(ks[11], (N_ODD, DIFF_VDIM)),
        "mla_q_norm_g": gain(ks[12], (N_ODD, MLA_Q_RANK)),
        "w_mla_uq": nrm(ks[13], (N_ODD, MLA_Q_RANK, MLA_HEADS * (MLA_NOPE + MLA_ROPE)), MLA_Q_RANK),
        "mla_kv_norm_g": gain(ks[14], (N_ODD, MLA_KV_RANK)),
        "w_mla_ukv": nrm(ks[15], (N_ODD, MLA_KV_RANK, MLA_HEADS * (MLA_NOPE + MLA_VDIM)), MLA_KV_RANK),
        "w_out_odd": nrm(ks[16], (N_ODD, ODD_MIX, D_MODEL), ODD_MIX),
        "t5_bias": 0.5 * jax.random.normal(ks[17], (T5_BUCKETS, T5_HEADS), F32),
        "w_ffn_gate": nrm(ks[18], (DEPTH, D_MODEL, FFN_HIDDEN), D_MODEL),
        "w_ffn_up": nrm(ks[19], (DEPTH, D_MODEL, FFN_HIDDEN), D_MODEL),
        "w_ffn_down": nrm(ks[20], (DEPTH, FFN_HIDDEN, D_MODEL), FFN_HIDDEN),
        "final_norm_g": gain(ks[21], (D_MODEL,)),
    }


def reference(x, norm_mix_g, norm_ffn_g, w_in_even, b_forget, w_out_even, w_in_odd,
              lambda_q1, lambda_k1, lambda_q2, lambda_k2, diff_subln_g, mla_q_norm_g,
              w_mla_uq, mla_kv_norm_g, w_mla_ukv, w_out_odd, t5_bias,
              w_ffn_gate, w_ffn_up, w_ffn_down, final_norm_g):
    for layer in range(DEPTH):
        j = layer // 2
        h = rms_norm(x, norm_mix_g[layer])
        if layer % 2 == 0:
            x = x + even_mixer(h, w_in_even[j], b_forget[j], w_out_even[j], t5_bias)
        else:
            lambda_init = 0.8 - 0.6 * math.exp(-0.3 * layer)
            x = x + odd_mixer(h, w_in_odd[j], lambda_q1[j], lambda_k1[j], lambda_q2[j],
                              lambda_k2[j], diff_subln_g[j], mla_q_norm_g[j], w_mla_uq[j],
                              mla_kv_norm_g[j], w_mla_ukv[j], w_out_odd[j], t5_bias,
                              lambda_init)
        h = rms_norm(x, norm_ffn_g[layer])
        x = x + swiglu(h, w_ffn_gate[layer], w_ffn_up[layer], w_ffn_down[layer])
    return rms_norm(x, final_norm_g)
```

```python
import math
from contextlib import ExitStack

import numpy as np
import concourse.bass as bass
import concourse.mybir as mybir
from concourse.bass_utils import run_bass_kernel_spmd

F32 = mybir.dt.float32
BF16 = mybir.dt.bfloat16
AF = mybir.ActivationFunctionType
ALU = mybir.AluOpType
AX = mybir.AxisListType

D = 1024
FFN = 2816
NEG = -30000.0
EPS = 1e-6
TOPK = 256
NIT = 16


class Res:
    __slots__ = ("w", "r")

    def __init__(self):
        self.w = {}
        self.r = {}


class TB:
    def __init__(self, h):
        self.h = h
        self.res = Res()


class Rec:
    def __init__(self):
        self.rec = None

    def __getattr__(self, name):
        def f(*a, **k):
            self.rec = (name, a, k)
            return self
        return f


class EngQ:
    def __init__(self, name, semid):
        self.name = name
        self.semid = semid
        self.cnt = 0
        self.ops = []
        self.waited = {}


class Sched:
    def __init__(self, nc, es):
        self.nc = nc
        self.sems = []
        self.engs = {}
        for name in ("pe", "act", "dve", "pool", "sp"):
            self.sems.append(es.enter_context(nc.semaphore("s_" + name)))
            self.engs[name] = EngQ(name, len(self.sems) - 1)
        self.dq = {}
        for qn, k in (("sp", 12), ("act", 6), ("pool", 8)):
            lst = []
            for i in range(k):
                self.sems.append(es.enter_context(nc.semaphore("d_%s%d" % (qn, i))))
                lst.append([len(self.sems) - 1, 0])
            self.dq[qn] = [lst, 0]

    def _wait(self, q, deps):
        for s, v in deps.items():
            if q.waited.get(s, 0) < v:
                q.ops.append(("w", s, v))
                q.waited[s] = v

    @staticmethod
    def _merge(d, s):
        for k, v in s.items():
            if d.get(k, 0) < v:
                d[k] = v

    def _deps(self, reads, writes, partial):
        deps = {}
        for R in reads:
            self._merge(deps, R.w)
        for R in writes:
            self._merge(deps, R.r)
            if not partial:
                self._merge(deps, R.w)
        return deps

    def _record(self, ev, reads, writes, partial):
        for R in writes:
            if partial:
                self._merge(R.w, ev)
            else:
                R.w = dict(ev)
                R.r = {}
        for R in reads:
            self._merge(R.r, ev)

    def op(self, eng, fn, reads=(), writes=(), partial=False):
        q = self.engs[eng]
        reads = [r.res if isinstance(r, TB) else r for r in reads]
        writes = [r.res if isinstance(r, TB) else r for r in writes]
        deps = self._deps(reads, writes, partial)
        if eng == "pe":
            deps.pop(q.semid, None)
        self._wait(q, deps)
        q.cnt += 1
        r = Rec()
        fn(r)
        q.ops.append(("i", r.rec))
        self._record({q.semid: q.cnt}, reads, writes, partial)

    def dma(self, queue, out, in_, reads=(), writes=(), partial=False, **kw):
        q = self.engs[queue]
        reads = [r.res if isinstance(r, TB) else r for r in reads]
        writes = [r.res if isinstance(r, TB) else r for r in writes]
        deps = self._deps(reads, writes, partial)
        lst, idx = self.dq[queue]
        slot = lst[idx]
        self.dq[queue][1] = (idx + 1) % len(lst)
        if slot[1] > 0:
            deps[slot[0]] = max(deps.get(slot[0], 0), slot[1])
        self._wait(q, deps)
        slot[1] += 16
        q.ops.append(("d", (lambda e, o=out, i=in_, k=kw: e.dma_start(out=o, in_=i, **k)), slot[0]))
        self._record({slot[0]: slot[1]}, reads, writes, partial)

    def barrier(self):
        tot = {}
        for q in self.engs.values():
            tot[q.semid] = q.cnt
        for lst, _ in self.dq.values():
            for s, v in lst:
                if v:
                    tot[s] = v
        for q in self.engs.values():
            d = {s: v for s, v in tot.items() if v > 0 and s != q.semid}
            self._wait(q, d)

    def emit(self):
        nc = self.nc
        sems = self.sems

        def run(q, e):
            own = sems[q.semid]
            for o in q.ops:
                if o[0] == "w":
                    e.wait_ge(sems[o[1]], o[2])
                elif o[0] == "i":
                    name, a, k = o[1]
                    getattr(e, name)(*a, **k).then_inc(own, 1)
                else:
                    o[1](e).then_inc(sems[o[2]], 16)
            q.ops = []

        with nc.Block() as block:
            @block.sync
            def _(e):
                run(self.engs["sp"], e)

            @block.tensor
            def _(e):
                run(self.engs["pe"], e)

            @block.scalar
            def _(e):
                run(self.engs["act"], e)

            @block.vector
            def _(e):
                run(self.engs["dve"], e)

            @block.gpsimd
            def _(e):
                run(self.engs["pool"], e)


def _t5_bucket_np(d):
    d = np.asarray(d)
    exact = 16
    df = np.maximum(d, 1).astype(np.float32)
    lb = exact + (np.log(df / np.float32(exact)) / np.float32(math.log(128 / exact))
                  * np.float32(32 - exact)).astype(np.int32)
    lb = np.minimum(lb, 31)
    return np.where(d < exact, d, lb)


def _consts(S):
    c = {}
    idx = np.arange(128)
    c["c_ident"] = np.eye(128, dtype=np.float32)
    c["c_ntriu"] = -(idx[:, None] <= idx[None, :]).astype(np.float32)
    col = np.arange(640)
    dd = col[None, :] - idx[:, None]
    c["c_cmstrip"] = np.where(dd >= 0, 0.0, NEG).astype(np.float32)
    c["c_cmtok"] = np.where(idx[None, :] <= idx[:, None], 0.0, NEG).astype(np.float32)
    pos = np.arange(S, dtype=np.float32)
    inv = (np.float32(10000.0) ** (-np.arange(0, 32, 2, dtype=np.float32) / np.float32(32))).astype(np.float32)
    ang = (pos[:, None] * inv[None, :]).astype(np.float32)
    ang = np.concatenate([ang, ang], axis=-1)
    cos = np.cos(ang).astype(np.float32).T
    sin = np.sin(ang).astype(np.float32).T
    c["c_cos4"] = np.ascontiguousarray(np.tile(cos, (4, 1)))
    c["c_sin4"] = np.ascontiguousarray(np.tile(sin, (4, 1)))
    bucket = _t5_bucket_np(np.maximum(dd, 0))
    return c, bucket


def build(S, layers=(0, 1), dbg=False):
    NB = S // 128
    NCH = S // 512
    nc = bass.Bass("TRN2", target_bir_lowering=False)

    def din(name, shape):
        return nc.dram_tensor(name, list(shape), F32, kind="ExternalInput").ap()

    def dscr(name, shape, dt=BF16):
        if dbg:
            return nc.dram_tensor(name, list(shape), dt, kind="ExternalOutput").ap()
        return nc.dram_tensor(name, list(shape), dt).ap()

    I = {}
    for name, shape in [
        ("x", [S, D]), ("norm_mix_g", [2, D]), ("norm_ffn_g", [2, D]),
        ("w_in_even", [D, 3404]), ("b_forget", [1, 8]), ("w_out_even", [D, D]),
        ("w_in_odd", [D, 1952]), ("lambda_q1", [1, 64]), ("lambda_k1", [1, 64]),
        ("lambda_q2", [1, 64]), ("lambda_k2", [1, 64]), ("diff_subln_g", [1, 128]),
        ("mla_q_norm_g", [1, 256]), ("w_mla_uq", [256, 768]), ("mla_kv_norm_g", [1, 128]),
        ("w_mla_ukv", [128, 1024]), ("w_out_odd", [D, D]), ("t5_31", [1, 8]),
        ("t5strip", [8, 128, 640]),
        ("w_ffn_gate", [2, D, FFN]), ("w_ffn_up", [2, D, FFN]), ("w_ffn_down", [2, FFN, D]),
        ("final_norm_g", [1, D]),
        ("c_ident", [128, 128]), ("c_ntriu", [128, 128]), ("c_cmstrip", [128, 640]),
        ("c_cmtok", [128, 128]), ("c_cos4", [128, S]), ("c_sin4", [128, S]),
    ]:
        I[name] = din(name, shape)
    y_out = nc.dram_tensor("y", [S, D], F32, kind="ExternalOutput").ap()

    qa = dscr("qa", [16, 96, S])
    ka = dscr("ka", [16, 96, S])
    vv = dscr("vv", [S, 16, 65])
    iqd = dscr("iqd", [4, 64, S])
    ikd = dscr("ikd", [64, S])
    mnegd = dscr("mnegd", [S, S])
    mixd = dscr("mixd", [S, D])
    xa = dscr("xa", [S, D], F32)
    xb = dscr("xb", [S, D], F32)

    top = ExitStack()
    sch = Sched(nc, top)
    op, dma = sch.op, sch.dma
    cnt = [0]

    def T(es, shape, dt):
        cnt[0] += 1
        return TB(es.enter_context(nc.sbuf_tensor("t%d" % cnt[0], list(shape), dt)))

    def PS(es, shape, dt=F32):
        cnt[0] += 1
        return TB(es.enter_context(nc.psum_tensor("p%d" % cnt[0], list(shape), dt)))

    identf = T(top, [128, 128], F32)
    identb = T(top, [128, 128], BF16)
    ntriu = T(top, [128, 128], F32)
    onesf = T(top, [128, 128], F32)
    cmtok = T(top, [128, 128], F32)
    cstrip = T(top, [128, 128], BF16)
    b31 = T(top, [128, 8], F32)
    iwabs = T(top, [128, NB, 4], F32)
    isgn = T(top, [128, NB, 4], F32)
    epsb = T(top, [128, 1], F32)

    def setup_consts():
        with ExitStack() as es:
            tmp = T(es, [128, 640], F32)
            dma("sp", identf.h[:], I["c_ident"], writes=[identf])
            dma("sp", ntriu.h[:], I["c_ntriu"], writes=[ntriu])
            dma("sp", cmtok.h[:], I["c_cmtok"], writes=[cmtok])
            dma("sp", b31.h[:], I["t5_31"].partition_broadcast(128), writes=[b31])
            dma("sp", tmp.h[:], I["c_cmstrip"], writes=[tmp])
            op("dve", lambda e: e.tensor_copy(out=identb.h[:], in_=identf.h[:]), [identf], [identb])
            op("dve", lambda e: e.memset(onesf.h[:], 1.0), [], [onesf])
            op("dve", lambda e: e.memset(epsb.h[:], EPS), [], [epsb])
            op("dve", lambda e: e.tensor_copy(out=cstrip.h[:], in_=tmp.h[:, 0:128]), [tmp], [cstrip])
            sch.barrier()
            sch.emit()

    def load_w(stg, dst, dst_k, dst_c0, src, r0, c0, cw, gs=None, gk=0, mul=None, ctr=[0], res=None):
        CW = stg[0].h.shape[1]
        for o in range(0, cw, CW):
            w = min(CW, cw - o)
            st = stg[ctr[0] % len(stg)]
            eng = "dve"
            ctr[0] += 1
            dma("sp", st.h[:, :w], src[r0:r0 + 128, c0 + o:c0 + o + w], writes=[st])
            dv = dst.h[:, dst_k, dst_c0 + o:dst_c0 + o + w]
            if gs is not None:
                rd = [st, gs]
                if mul is None:
                    f = lambda e, dv=dv, st=st, w=w: e.tensor_scalar(
                        out=dv, in0=st.h[:, :w], scalar1=gs.h[:, gk:gk + 1], scalar2=None, op0=ALU.mult)
                else:
                    f = lambda e, dv=dv, st=st, w=w: e.tensor_scalar(
                        out=dv, in0=st.h[:, :w], scalar1=gs.h[:, gk:gk + 1], scalar2=float(mul),
                        op0=ALU.mult, op1=ALU.mult)
            else:
                rd = [st]
                if mul is None:
                    f = lambda e, dv=dv, st=st, w=w: e.tensor_copy(out=dv, in_=st.h[:, :w])
                else:
                    f = lambda e, dv=dv, st=st, w=w: e.tensor_scalar(
                        out=dv, in0=st.h[:, :w], scalar1=float(mul), scalar2=None, op0=ALU.mult)
            op(eng, f, rd, [dst if res is None else res], partial=True)

    def load_gain(es, src_row, n):
        k = n // 128
        g = T(es, [128, k], F32)
        dma("sp", g.h[:], src_row.rearrange("(k p) -> p k", p=128), writes=[g],
            allow_slow_non_contiguous=True)
        return g

    def make_norm_T(es, nxt=3):
        st = {
            "xt": [T(es, [128, D], F32) for _ in range(nxt)],
            "junk": T(es, [128, D], BF16),
            "ssq": [T(es, [128, 1], F32) for _ in range(3)],
            "rstd": [T(es, [128, 1], F32) for _ in range(3)],
            "xn": [T(es, [128, D], BF16) for _ in range(2)],
            "tp": [PS(es, [128, 8, 128], BF16) for _ in range(2)],
            "i": 0,
        }
        return st

    def rstd_of(ssq, rstd, n, junk_unused=None):
        op("act", lambda e: e.activation(out=rstd.h[:], in_=ssq.h[:], func=AF.Sqrt, bias=epsb.h[:],
                                         scale=1.0 / n), [ssq, epsb], [rstd])
        op("dve", lambda e: e.reciprocal(out=rstd.h[:], in_=rstd.h[:]), [rstd], [rstd])

    def norm_T(st, xt, hT, b):
        i = st["i"]
        st["i"] += 1
        ssq, rstd, xn, tp = st["ssq"][i % 3], st["rstd"][i % 3], st["xn"][i % 2], st["tp"][i % 2]
        junk = st["junk"]
        op("act", lambda e: e.activation(out=junk.h[:], in_=xt.h[:], func=AF.Square, accum_out=ssq.h[:]),
           [xt], [junk, ssq])
        rstd_of(ssq, rstd, D)
        op("dve", lambda e: e.tensor_scalar(out=xn.h[:], in0=xt.h[:], scalar1=rstd.h[:], scalar2=None,
                                            op0=ALU.mult), [xt, rstd], [xn])
        for k in range(8):
            op("pe", lambda e, k=k: e.transpose(out=tp.h[:, k, :], in_=xn.h[:, 128 * k:128 * k + 128],
                                                identity=identb.h[:]), [xn, identb], [tp])
        op("act", lambda e: e.copy(out=hT.h[:, :, 128 * b:128 * b + 128], in_=tp.h[:]), [tp], [hT],
           partial=True)

    def fm_group(W, KC, col0, M, hT, ps, kbase=0, wres=None):
        for k in range(KC):
            op("pe", lambda e, k=k: e.matmul(ps.h[:M, :], W.h[:, k, col0:col0 + M], hT.h[:, kbase + k, :],
                                             start=(k == 0), stop=(k == KC - 1)), [W if wres is None else wres, hT], [ps])

    def phase_proj_even(xsrc):
        with ExitStack() as es:
            g = load_gain(es, I["norm_mix_g"][0], D)
            W = T(es, [128, 8, 3404], BF16)
            stg = [T(es, [128, 1702], F32) for _ in range(3)]
            cmap = [(0, 0, 512, 0.125), (512, 512, 512, None), (1544, 1024, 512, 0.125), (2056, 1536, 512, None),
                    (3080, 2048, 256, 0.125 * 0.5), (3336, 2304, 64, None),
                    (1024, 2368, 512, None), (2568, 2880, 512, None), (1536, 3392, 8, None), (3400, 3400, 4, None)]
            wres = [Res() for _ in cmap]
            bounds = [dc for (_, dc, _, _) in cmap]

            def went(col):
                e = 0
                for i_, d_ in enumerate(bounds):
                    if col >= d_:
                        e = i_
                return wres[e]
            for ei, (sc, dc, cw, mul) in enumerate(cmap):
                for k in range(8):
                    load_w(stg, W, k, dc, I["w_in_even"], 128 * k, sc, cw, gs=g, gk=k, mul=mul, res=wres[ei])
            nst = make_norm_T(es, 8)
            hTs = [T(es, [128, 8, 512], BF16) for _ in range(2)]
            pfm = [PS(es, [128, 512]) for _ in range(2)]
            ptm = [PS(es, [128, 512]) for _ in range(2)]
            psm = PS(es, [128, 512])
            pct = PS(es, [128, 128], BF16)
            fst = [T(es, [128, 512], BF16) for _ in range(6)]
            vst = [T(es, [128, 16, 65], BF16) for _ in range(2)]
            bfb = T(es, [128, 8], F32)
            dma("sp", bfb.h[:], I["b_forget"].partition_broadcast(128), writes=[bfb])
            carry = T(es, [1, 8], F32)
            op("dve", lambda e: e.memset(carry.h[:], 0.0), [], [carry])
            for v in vst:
                op("pool", lambda e, v=v: e.memset(v.h[:], 1.0), [], [v])
            onesb = T(es, [128, 512], BF16)
            op("pool", lambda e: e.memset(onesb.h[:], 1.0), [], [onesb])
            for h in range(8):
                for c in range(NCH):
                    dma("pool", qa[h, 67:70, 512 * c:512 * c + 512], onesb.h[0:3, :], reads=[onesb])
                    dma("pool", ka[h, 64:67, 512 * c:512 * c + 512], onesb.h[0:3, :], reads=[onesb])
            zerob = T(es, [8, 512], BF16)
            op("pool", lambda e: e.memset(zerob.h[:], 0.0), [], [zerob])
            for h in range(8, 16):
                for c in range(NCH):
                    dma("pool", qa[h, 64:72, 512 * c:512 * c + 512], zerob.h[:, :], reads=[zerob])
                    dma("pool", ka[h, 64:72, 512 * c:512 * c + 512], zerob.h[:, :], reads=[zerob])
            xb8 = T(es, [128, 8], F32)
            ee = T(es, [128, 8], F32)
            ll = T(es, [128, 8], F32)
            c32 = T(es, [128, 8], F32)
            t32 = T(es, [128, 8], F32)
            r1 = T(es, [128, 8], F32)
            CT = T(es, [128, 8, 6], BF16)
            CTs = [T(es, [48, 512], BF16) for _ in range(2)]
            tmp4 = T(es, [128, 4], F32)
            gi = 0
            def load_x(c):
                for b in range(4):
                    blk = 4 * c + b
                    xt = nst["xt"][blk % 8]
                    dma("sp", xt.h[:], xsrc[128 * blk:128 * blk + 128, :], writes=[xt])
            load_x(0)
            for c in range(NCH):
                hT = hTs[c % 2]
                cts = CTs[c % 2]
                if c + 1 < NCH:
                    load_x(c + 1)
                for b in range(4):
                    blk = 4 * c + b
                    xt = nst["xt"][blk % 8]
                    norm_T(nst, xt, hT, b)
                for gidx in range(19):
                    col0 = 128 * gidx
                    M = min(128, 2368 - col0)
                    ps = pfm[gi % 2]
                    st = fst[gi % 6]
                    gi += 1
                    fm_group(W, 8, col0, M, hT, ps, wres=went(col0))
                    if gi % 2 == 0:
                        op("act", lambda e, st=st, ps=ps, M=M: e.copy(out=st.h[:M, :], in_=ps.h[:M, :]), [ps], [st])
                    else:
                        op("dve", lambda e, st=st, ps=ps, M=M: e.tensor_copy(out=st.h[:M, :], in_=ps.h[:M, :]),
                           [ps], [st])
                    cs = slice(512 * c, 512 * c + 512)
                    for half in range(M // 64):
                        hd = 2 * gidx + half
                        src = st.h[64 * half:64 * half + 64, :]
                        if hd < 8:
                            dst = qa[hd, 0:64, cs]
                        elif hd < 16:
                            dst = ka[hd - 8, 0:64, cs]
                        elif hd < 24:
                            dst = qa[hd - 8, 0:64, cs]
                        elif hd < 32:
                            dst = ka[hd - 16, 0:64, cs]
                        elif hd < 36:
                            dst = iqd[hd - 32, :, cs]
                        else:
                            dst = ikd[:, cs]
                        dma("sp", dst, src, reads=[st])
                for b in range(4):
                    blk = 4 * c + b
                    tok = slice(128 * blk, 128 * blk + 128)
                    vs = vst[blk % 2]
                    for vi in range(2):
                        ps = ptm[vi]
                        for k in range(8):
                            op("pe", lambda e, k=k, ps=ps, vi=vi: e.matmul(
                                ps.h[:, :], hT.h[:, k, 128 * b:128 * b + 128],
                                W.h[:, k, 2368 + 512 * vi:2368 + 512 * vi + 512],
                                start=(k == 0), stop=(k == 7)), [went(2368 + 512 * vi), hT], [ps])
                        eng = "act" if vi == 0 else "dve"
                        if eng == "act":
                            op("act", lambda e, ps=ps, vs=vs, vi=vi: e.copy(
                                out=vs.h[:, 8 * vi:8 * vi + 8, 0:64],
                                in_=ps.h[:, :].rearrange("p (h d) -> p h d", d=64)), [ps], [vs], partial=True)
                        else:
                            op("dve", lambda e, ps=ps, vs=vs, vi=vi: e.tensor_copy(
                                out=vs.h[:, 8 * vi:8 * vi + 8, 0:64],
                                in_=ps.h[:, :].rearrange("p (h d) -> p h d", d=64)), [ps], [vs], partial=True)
                    dma("pool", vv[tok, :, :], vs.h[:], reads=[vs])
                    for k in range(8):
                        op("pe", lambda e, k=k: e.matmul(psm.h[:, 0:12], hT.h[:, k, 128 * b:128 * b + 128],
                                                         W.h[:, k, 3392:3404], start=(k == 0), stop=(k == 7)),
                           [wres[8], wres[9], hT], [psm])
                    op("dve", lambda e: e.tensor_scalar(out=tmp4.h[:], in0=psm.h[:, 8:12], scalar1=0.0, scalar2=2.0,
                                                        op0=ALU.is_ge, op1=ALU.mult), [psm], [tmp4])
                    op("dve", lambda e, blk=blk: e.tensor_scalar_add(out=isgn.h[:, blk, :], in0=tmp4.h[:],
                                                                     scalar1=-1.0), [tmp4], [isgn], partial=True)
                    op("dve", lambda e, blk=blk: e.tensor_tensor(out=iwabs.h[:, blk, :], in0=psm.h[:, 8:12],
                                                                 in1=isgn.h[:, blk, :], op=ALU.mult),
                       [psm, isgn], [iwabs], partial=True)
                    op("dve", lambda e: e.tensor_tensor(out=xb8.h[:], in0=psm.h[:, 0:8], in1=bfb.h[:], op=ALU.add),
                       [psm, bfb], [xb8])
                    op("act", lambda e: e.activation(out=ee.h[:], in_=xb8.h[:], func=AF.Exp, scale=-1.0),
                       [xb8], [ee])
                    op("act", lambda e: e.activation(out=ll.h[:], in_=ee.h[:], func=AF.Ln, bias=1.0),
                       [ee], [ll])
                    op("pe", lambda e: e.matmul(psm.h[:, 16:24], ntriu.h[:], ll.h[:], start=True, stop=False),
                       [ntriu, ll], [psm])
                    op("pe", lambda e: e.matmul(psm.h[:, 16:24], onesf.h[0:1, :], carry.h[0:1, :], start=False,
                                                stop=True), [onesf, carry], [psm])
                    op("pe", lambda e: e.matmul(psm.h[0:1, 32:40], onesf.h[:, 0:1], ll.h[:], start=True, stop=True),
                       [onesf, ll], [psm])
                    op("dve", lambda e: e.tensor_copy(out=c32.h[:], in_=psm.h[:, 16:24]), [psm], [c32])
                    op("dve", lambda e: e.tensor_tensor(out=carry.h[:], in0=carry.h[:], in1=psm.h[0:1, 32:40],
                                                        op=ALU.subtract), [carry, psm], [carry])
                    op("dve", lambda e: e.tensor_copy(out=CT.h[:, :, 0], in_=c32.h[:]), [c32], [CT], partial=True)
                    op("dve", lambda e: e.tensor_copy(out=t32.h[:], in_=CT.h[:, :, 0]), [CT], [t32])
                    op("dve", lambda e: e.tensor_tensor(out=r1.h[:], in0=c32.h[:], in1=t32.h[:], op=ALU.subtract),
                       [c32, t32], [r1])
                    op("dve", lambda e: e.tensor_copy(out=CT.h[:, :, 1], in_=r1.h[:]), [r1], [CT], partial=True)
                    op("dve", lambda e: e.tensor_copy(out=t32.h[:], in_=CT.h[:, :, 1]), [CT], [t32])
                    op("dve", lambda e: e.tensor_tensor(out=r1.h[:], in0=r1.h[:], in1=t32.h[:], op=ALU.subtract),
                       [r1, t32], [r1])
                    op("dve", lambda e: e.tensor_copy(out=CT.h[:, :, 2], in_=r1.h[:]), [r1], [CT], partial=True)
                    op("dve", lambda e: e.tensor_scalar(out=CT.h[:, :, 3:6], in0=CT.h[:, :, 0:3], scalar1=-1.0,
                                                        scalar2=None, op0=ALU.mult), [CT], [CT], partial=True)
                    op("pe", lambda e: e.transpose(out=pct.h[0:48, :], in_=CT.h[:].rearrange("p h s -> p (h s)"),
                                                   identity=identb.h[:]), [CT, identb], [pct])
                    op("dve", lambda e, b=b, cts=cts: e.tensor_copy(out=cts.h[:, 128 * b:128 * b + 128],
                                                                   in_=pct.h[0:48, :]), [pct], [cts], partial=True)
                cs = slice(512 * c, 512 * c + 512)
                for h in range(8):
                    dma("pool", qa[h, 64:67, cs], cts.h[6 * h:6 * h + 3, :], reads=[cts])
                    dma("pool", ka[h, 67:70, cs], cts.h[6 * h + 3:6 * h + 6, :], reads=[cts])
            sch.barrier()
            sch.emit()

    def phase_index():
        with ExitStack() as es:
            iq = T(es, [128, 2, S], BF16)
            ik = T(es, [128, S], BF16)
            for h in range(4):
                dma("sp", iq.h[64 * (h % 2):64 * (h % 2) + 64, h // 2, :], iqd[h], writes=[iq], partial=True)
            for r in range(2):
                dma("sp", ik.h[64 * r:64 * r + 64, :], ikd, writes=[ik], partial=True)
            SCs = [T(es, [128, S], F32) for _ in range(2)]
            junks = [T(es, [128, S], BF16) for _ in range(2)]
            MNs = [T(es, [128, S], BF16) for _ in range(2)]
            Rt = [T(es, [128, 512], BF16) for _ in range(8)]
            Dg = [T(es, [128, 128], BF16) for _ in range(8)]
            X = [PS(es, [128, 512]) for _ in range(4)]
            SP_ = [PS(es, [128, 512]) for _ in range(2)]
            TPm = [PS(es, [128, 8, 128], BF16) for _ in range(2)]
            tst = [T(es, [128, 8, 128], BF16) for _ in range(3)]
            pw = T(es, [128, NIT + 1], F32)
            for it in range(NIT + 1):
                op("pool", lambda e, it=it: e.memset(pw.h[:, it:it + 1], float(2.0 ** (-(it + 1)))), [], [pw],
                   partial=True)
            sm = [[T(es, [128, 1], F32) for _ in range(8)] for _ in range(2)]
            stp = [T(es, [128, NIT + 1], F32) for _ in range(2)]
            nstp = [T(es, [128, NIT + 1], F32) for _ in range(2)]
            cn = {"ri": 0, "si": 0, "ti": 0, "tp": 0}

            def scores(i, SC):
                n = 128 * (i + 1)
                q0 = 128 * i
                dset = Dg[4 * (i % 2):4 * (i % 2) + 4]
                for h in range(4):
                    op("dve", lambda e, h=h: e.tensor_scalar(
                        out=dset[h].h[:], in0=identb.h[:], scalar1=isgn.h[:, i, h:h + 1], scalar2=None, op0=ALU.mult),
                       [identb, isgn], [dset[h]])
                nk = (n + 511) // 512
                pend = None
                for kc in range(nk + 1):
                    cur = None
                    if kc < nk:
                        k0 = 512 * kc
                        kw = min(512, n - k0)
                        rs = []
                        for h in range(4):
                            pb = 64 * (h % 2)
                            op("pe", lambda e, h=h, pb=pb: e.matmul(
                                X[h].h[:, :kw], iq.h[pb:pb + 64, h // 2, q0:q0 + 128], ik.h[pb:pb + 64, k0:k0 + kw],
                                start=True, stop=True), [iq, ik], [X[h]])
                            r = Rt[cn["ri"] % 8]
                            cn["ri"] += 1
                            rs.append(r)
                            if h < 2:
                                op("act", lambda e, h=h, r=r: e.activation(
                                    out=r.h[:, :kw], in_=X[h].h[:, :kw], func=AF.Relu, scale=iwabs.h[:, i, h:h + 1]),
                                   [X[h], iwabs], [r])
                            else:
                                op("dve", lambda e, h=h, r=r: e.tensor_scalar(
                                    out=r.h[:, :kw], in0=X[h].h[:, :kw], scalar1=iwabs.h[:, i, h:h + 1], scalar2=0.0,
                                    op0=ALU.mult, op1=ALU.max), [X[h], iwabs], [r])
                        cur = (k0, kw, rs)
                    if pend is not None:
                        pk0, pkw, prs = pend
                        sp = SP_[cn["si"] % 2]
                        cn["si"] += 1
                        for h in range(4):
                            op("pe", lambda e, h=h: e.matmul(
                                sp.h[:, :pkw], dset[h].h[:], prs[h].h[:, :pkw], start=(h == 0), stop=(h == 3)),
                               [dset[h], prs[h]], [sp])
                        op("act", lambda e: e.copy(out=SC.h[:, pk0:pk0 + pkw], in_=sp.h[:, :pkw]), [sp], [SC],
                           partial=True)
                    pend = cur

            def prep(i, SC, st, ch):
                n = 128 * (i + 1)
                A, lo, mid, cntt, ge, u2, tq, _ = sm[ch]
                op("dve", lambda e: e.tensor_reduce(out=A.h[:], in_=SC.h[:, 0:n], axis=AX.X, op=ALU.max,
                                                    apply_absolute_value=True), [SC], [A])
                op("dve", lambda e: e.tensor_scalar(out=u2.h[:], in0=A.h[:], scalar1=1.0, scalar2=2.0,
                                                    op0=ALU.add, op1=ALU.mult), [A], [u2])
                op("dve", lambda e: e.tensor_scalar(out=lo.h[:], in0=u2.h[:], scalar1=-0.5, scalar2=None,
                                                    op0=ALU.mult), [u2], [lo])
                op("dve", lambda e: e.tensor_scalar(out=stp[ch].h[:], in0=pw.h[:], scalar1=u2.h[:], scalar2=None,
                                                    op0=ALU.mult), [pw, u2], [stp[ch]])
                op("dve", lambda e: e.memset(mid.h[:], 0.0), [], [mid])
                if ch == 1:
                    op("dve", lambda e: e.tensor_scalar(out=nstp[ch].h[:], in0=stp[ch].h[:], scalar1=-1.0, scalar2=None,
                                                        op0=ALU.mult), [stp[ch]], [nstp[ch]])
                    op("dve", lambda e: e.memset(tq.h[:], 0.0), [], [tq])
                op("pool", lambda e: e.tensor_tensor(out=SC.h[:, n - 128:n], in0=SC.h[:, n - 128:n],
                                                     in1=cmtok.h[:], op=ALU.add), [SC, cmtok], [SC])

            def iter_dve(i, SC, it):
                n = 128 * (i + 1)
                A, lo, mid, cntt, ge, u2, tq, _ = sm[0]
                junk = junks[0]
                op("dve", lambda e: e.tensor_scalar(out=junk.h[:, 0:n], in0=SC.h[:, 0:n], scalar1=mid.h[:], scalar2=0.0,
                                                    op0=ALU.is_ge, op1=ALU.add, accum_out=cntt.h[:]),
                   [SC, mid], [junk, cntt])
                op("dve", lambda e: e.tensor_scalar(out=ge.h[:], in0=cntt.h[:], scalar1=float(TOPK) - 0.5, scalar2=0.5,
                                                    op0=ALU.is_ge, op1=ALU.subtract), [cntt], [ge])
                op("dve", lambda e: e.scalar_tensor_tensor(out=mid.h[:], in0=ge.h[:], scalar=stp[0].h[:, it:it + 1],
                                                           in1=mid.h[:], op0=ALU.mult, op1=ALU.add),
                   [ge, stp[0], mid], [mid])
                if it == NIT - 1:
                    op("dve", lambda e: e.tensor_tensor(out=lo.h[:], in0=mid.h[:], in1=stp[0].h[:, NIT:NIT + 1],
                                                        op=ALU.subtract), [mid, stp[0]], [lo])

            def iter_act(i, SC, it):
                n = 128 * (i + 1)
                A, lo, nmA, cs, sg, u2, nmB, _ = sm[1]
                nm_cur, nm_nxt = (nmB, nmA) if it % 2 == 0 else (nmA, nmB)
                junk = junks[1]
                op("act", lambda e: e.activation(out=junk.h[:, 0:n], in_=SC.h[:, 0:n], func=AF.Sign, bias=nm_cur.h[:],
                                                 accum_out=cs.h[:]), [SC, nm_cur], [junk, cs])
                op("act", lambda e: e.activation(out=sg.h[:], in_=cs.h[:], func=AF.Sign,
                                                 bias=float(n - 2 * TOPK) + 0.5), [cs], [sg])
                op("act", lambda e: e.activation(out=nm_nxt.h[:], in_=sg.h[:], func=AF.Identity,
                                                 scale=nstp[1].h[:, it + 1:it + 2], bias=nm_cur.h[:]),
                   [sg, nstp[1], nm_cur], [nm_nxt])
                if it == NIT - 1:
                    op("act", lambda e: e.activation(out=lo.h[:], in_=nm_nxt.h[:], func=AF.Identity, scale=-1.0,
                                                     bias=nstp[1].h[:, NIT:NIT + 1]), [nm_nxt, nstp[1]], [lo])

            def finish(i, SC, ch):
                n = 128 * (i + 1)
                q0 = 128 * i
                lo = sm[ch][1]
                mn = MNs[ch]
                op("dve", lambda e: e.tensor_scalar(out=mn.h[:, 0:n], in0=SC.h[:, 0:n], scalar1=lo.h[:], scalar2=None,
                                                    op0=ALU.is_ge), [SC, lo], [mn])
                for j0 in range(0, i + 1, 8):
                    nj = min(8, i + 1 - j0)
                    tp = TPm[cn["tp"] % 2]
                    cn["tp"] += 1
                    ts_ = tst[cn["ti"] % 3]
                    cn["ti"] += 1
                    for jj in range(nj):
                        j = j0 + jj
                        op("pe", lambda e, jj=jj, j=j: e.transpose(out=tp.h[:, jj, :], in_=mn.h[:, 128 * j:128 * j + 128],
                                                                   identity=identb.h[:]), [mn, identb], [tp])
                    op("act", lambda e: e.copy(out=ts_.h[:, 0:nj, :], in_=tp.h[:, 0:nj, :]), [tp], [ts_])
                    dma("sp", mnegd[128 * j0:128 * (j0 + nj), q0:q0 + 128].rearrange("(j p) t -> p j t", p=128),
                        ts_.h[:, 0:nj, :], reads=[ts_])

            for i0 in range(0, NB, 2):
                ia, ib = i0, i0 + 1
                scores(ib, SCs[1])
                scores(ia, SCs[0])
                prep(ib, SCs[1], None, 1)
                prep(ia, SCs[0], None, 0)
                for it in range(NIT):
                    if 128 * (ib + 1) > TOPK:
                        iter_act(ib, SCs[1], it)
                    if 128 * (ia + 1) > TOPK:
                        iter_dve(ia, SCs[0], it)
                finish(ib, SCs[1], 1)
                finish(ia, SCs[0], 0)
            sch.barrier()
            sch.emit()

    def phase_attn(maps, Kd, G, final, use_mneg=False, nunits=1, t5=False, prefetch=True):
        groups_ = [maps[g0:g0 + G] for g0 in range(0, len(maps), G)]
        nu = max(len(set(u for m in grp for u in m["vu"])) for grp in groups_)
        NKV = 2 if prefetch else 1
        with ExitStack() as es:
            Kts = [T(es, [128, G, S], BF16) for _ in range(NKV)]
            Vts = [T(es, [128, NB, nu, 65], BF16) for _ in range(NKV)]

            def load_kv(gidx):
                grp = groups_[gidx]
                Kt, Vt = Kts[gidx % NKV], Vts[gidx % NKV]
                units = sorted(set(u for m in grp for u in m["vu"]))
                for gi, m in enumerate(grp):
                    dma("sp", Kt.h[:Kd, gi, :], ka[m["ki"], 0:Kd, :], writes=[Kt], partial=True)
                for ui, u in enumerate(units):
                    for n0 in range(0, NB, 8):
                        n1 = min(NB, n0 + 8)
                        dma("sp", Vt.h[:, n0:n1, ui, :],
                            vv[128 * n0:128 * n1, u, :].rearrange("(n p) d -> p n d", p=128),
                            writes=[Vt], partial=True)

            Qt = [T(es, [128, G, 512], BF16) for _ in range(2)]
            NET = 4
            Et = [T(es, [128, 1024], BF16) for _ in range(NET)]
            NSP = 3
            Sp = [PS(es, [128, 1024]) for _ in range(NSP)]
            Op = [PS(es, [128, 512]) for _ in range(nunits)]
            if nunits == 1:
                Tp = PS(es, [128, 4, 128])
            else:
                Tp = TB(Sp[NSP - 1].h[:, 0:512].rearrange("p (u d) -> p u d", d=128))
                Tp.res = Sp[NSP - 1].res
            oT = [T(es, [65, 512], F32) for _ in range(2 * nunits)]
            if use_mneg:
                Mt = [T(es, [128, 8, 512], BF16) for _ in range(NB // 8)]
            fst = final["alloc"](es)
            cnts = {"s": 0, "e": 0, "o": 0}
            strips = None
            if t5:
                strips = T(es, [128, 8, 640], BF16)
                tmpa = T(es, [128, 640], F32)
                tmpb = T(es, [128, 640], F32)
                dma("sp", tmpa.h[:], I["c_cmstrip"], writes=[tmpa])
                for h in range(8):
                    dma("sp", tmpb.h[:], I["t5strip"][h], writes=[tmpb])
                    op("dve", lambda e, h=h: e.tensor_tensor(out=strips.h[:, h, :], in0=tmpb.h[:], in1=tmpa.h[:],
                                                            op=ALU.add), [tmpb, tmpa], [strips], partial=True)
            if prefetch:
                load_kv(0)
            for gidx, grp in enumerate(groups_):
                Kt, Vt = Kts[gidx % NKV], Vts[gidx % NKV]
                units = sorted(set(u for m in grp for u in m["vu"]))
                if prefetch:
                    if gidx + 1 < len(groups_):
                        load_kv(gidx + 1)
                else:
                    load_kv(gidx)
                for c in range(NCH):
                    qt = Qt[c % 2]
                    for gi, m in enumerate(grp):
                        dma("sp", qt.h[:Kd, gi, :], qa[m["qi"], 0:Kd, 512 * c:512 * c + 512], writes=[qt],
                            partial=True)
                    if use_mneg:
                        nkb = 4 * c + 4
                        for p0 in range(0, nkb, 8):
                            npz = min(8, nkb - p0)
                            dma("sp", Mt[p0 // 8].h[:, 0:npz, :],
                                mnegd[128 * p0:128 * (p0 + npz), 512 * c:512 * c + 512].rearrange("(j p) t -> p j t", p=128),
                                writes=[Mt[p0 // 8]])
                    for gi, m in enumerate(grp):
                        ui_list = [units.index(u) for u in m["vu"]]
                        groups = []
                        if m["strip"] == "c":
                            sap = cstrip.h[:, :]
                            sres = cstrip
                        else:
                            sap = strips.h[:, m["strip"], :]
                            sres = strips
                        nfar = max(4 * c - 1, 0) if m["near"] else 4 * c
                        j = 0
                        while j < nfar:
                            if j + 1 < nfar:
                                groups.append([(j, 0, None), (j + 1, 0, None)])
                                j += 2
                            else:
                                groups.append([(j, 0, None)])
                                j += 1
                        for j in range(nfar, 4 * c + 4):
                            r = j - 4 * c
                            if r < 0:
                                groups.append([(j, 0, 128)])
                            else:
                                groups.append([(j, 128 * r, 0)])
                        last_j = 4 * c + 3

                        def emit_qk(grpb):
                            sp = Sp[cnts["s"] % NSP]
                            cnts["s"] += 1
                            for bi, (j, col0, off) in enumerate(grpb):
                                base = 512 * bi
                                N = 512 - col0
                                extra = (off is not None)
                                op("pe", lambda e, sp=sp, j=j, col0=col0, N=N, base=base, extra=extra: e.matmul(
                                    sp.h[:, base + col0:base + 512], Kt.h[:Kd, gi, 128 * j:128 * j + 128],
                                    qt.h[:Kd, gi, col0:512], start=True, stop=not extra), [Kt, qt], [sp])
                                if off is not None:
                                    NN = N if m["near"] else 128
                                    op("pe", lambda e, sp=sp, col0=col0, NN=NN, base=base, off=off: e.matmul(
                                        sp.h[:, base + col0:base + col0 + NN], identb.h[:], sap[:, off:off + NN],
                                        start=False, stop=True), [identb, sres], [sp])
                            return sp

                        def emit_exp(grpb, sp):
                            et = Et[cnts["e"] % NET]
                            cnts["e"] += 1
                            col0 = grpb[0][1]
                            far = grpb[0][2] is None
                            bias = m["bias"] if (far and m["bias"] is not None) else None
                            if len(grpb) == 2:
                                src, dst = sp.h[:, 0:1024], et.h[:, 0:1024]
                            else:
                                src, dst = sp.h[:, col0:512], et.h[:, col0:512]
                            if bias is not None:
                                op("act", lambda e, src=src, dst=dst, bias=bias: e.activation(
                                    out=dst, in_=src, func=AF.Exp, bias=bias), [sp, b31], [et])
                            else:
                                op("act", lambda e, src=src, dst=dst: e.activation(out=dst, in_=src, func=AF.Exp),
                                   [sp], [et])
                            if use_mneg:
                                j = grpb[0][0]
                                mt = Mt[j // 8]
                                if len(grpb) == 2:
                                    msk = mt.h[:, j % 8:j % 8 + 2, :].rearrange("p j t -> p (j t)")
                                else:
                                    msk = mt.h[:, j % 8, col0:512]
                                meng = "dve"
                                op(meng, lambda e, dst=dst, msk=msk: e.tensor_tensor(out=dst, in0=dst, in1=msk,
                                                                                      op=ALU.mult), [et, mt], [et])
                            return et

                        def emit_pv(grpb, et):
                            for bi, (j, col0, off) in enumerate(grpb):
                                base = 512 * bi
                                for k, ui in enumerate(ui_list):
                                    op("pe", lambda e, j=j, col0=col0, base=base, k=k, ui=ui: e.matmul(
                                        Op[k].h[0:65, col0:512], Vt.h[:, j, ui, :], et.h[:, base + col0:base + 512],
                                        start=(j == 0), stop=(j == last_j)), [Vt, et], [Op[k]])

                        sps = [None] * len(groups)
                        LA = NSP - 1
                        PVLAG = 1
                        pend_pv = []
                        for t in range(min(LA, len(groups))):
                            sps[t] = emit_qk(groups[t])
                        for t in range(len(groups)):
                            if t + LA < len(groups):
                                sps[t + LA] = emit_qk(groups[t + LA])
                            et = emit_exp(groups[t], sps[t])
                            pend_pv.append((groups[t], et))
                            if len(pend_pv) > PVLAG:
                                emit_pv(*pend_pv.pop(0))
                        while pend_pv:
                            emit_pv(*pend_pv.pop(0))
                        tps = []
                        for k in range(len(ui_list)):
                            o = oT[cnts["o"] % len(oT)]
                            cnts["o"] += 1
                            op("dve", lambda e, o=o, k=k: e.tensor_copy(out=o.h[:, :], in_=Op[k].h[0:65, :]),
                               [Op[k]], [o])
                            tps.append(o)
                        final["fn"](fst, m, c, tps, Tp)
                if not prefetch:
                    sch.barrier()
                sch.emit()
            sch.barrier()
            sch.emit()

    def std_final():
        def alloc(es):
            return {"rec": [T(es, [128, 4, 1], F32) for _ in range(2)],
                    "stg": [T(es, [128, 4, 64], BF16) for _ in range(2)], "i": 0}

        def fn(st, m, c, tps, Tp):
            i = st["i"]
            st["i"] += 1
            rec, stg = st["rec"][i % 2], st["stg"][i % 2]
            o = tps[0]
            for u in range(4):
                op("pe", lambda e, u=u: e.transpose(out=Tp.h[:, u, 0:65], in_=o.h[0:65, 128 * u:128 * u + 128],
                                                    identity=identf.h[0:65, 0:65]), [o, identf], [Tp])
            op("dve", lambda e: e.reciprocal(out=rec.h[:], in_=Tp.h[:, :, 64:65]), [Tp], [rec])
            op("dve", lambda e: e.tensor_tensor(out=stg.h[:], in0=Tp.h[:, :, 0:64],
                                                in1=rec.h[:].to_broadcast([128, 4, 64]), op=ALU.mult),
               [Tp, rec], [stg])
            col = m["col"]
            dma("pool", mixd[512 * c:512 * c + 512, col:col + 64].rearrange("(u p) d -> p u d", p=128), stg.h[:],
                reads=[stg])
        return {"alloc": alloc, "fn": fn}

    def phase_proj_odd(xsrc):
        QS = 96.0 ** -0.5
        with ExitStack() as es:
            g = load_gain(es, I["norm_mix_g"][1], D)
            gq = load_gain(es, I["mla_q_norm_g"][0], 256)
            gkv = load_gain(es, I["mla_kv_norm_g"][0], 128)
            W = T(es, [128, 8, 1984], BF16)
            Wuq = T(es, [128, 2, 1024], BF16)
            Wukv = T(es, [128, 1, 1024], BF16)
            stg = [T(es, [128, 512], F32) for _ in range(3)]
            src = I["w_in_odd"]
            for k in range(8):
                r0 = 128 * k
                load_w(stg, W, k, 0, src, r0, 0, 512, gs=g, gk=k, mul=0.125)
                load_w(stg, W, k, 512, src, r0, 512, 512, gs=g, gk=k)
                load_w(stg, W, k, 1024, src, r0, 1920, 32, gs=g, gk=k)
                load_w(stg, W, k, 1056, src, r0, 1936, 16, gs=g, gk=k, mul=-1.0)
                load_w(stg, W, k, 1072, src, r0, 1920, 16, gs=g, gk=k)
                load_w(stg, W, k, 1088, src, r0, 1024, 512, gs=g, gk=k)
                load_w(stg, W, k, 1600, src, r0, 1536, 384, gs=g, gk=k)
            for k in range(2):
                r0 = 128 * k
                for h in range(8):
                    load_w(stg, Wuq, k, 64 * h, I["w_mla_uq"], r0, 96 * h, 64, gs=gq, gk=k, mul=QS)
                    load_w(stg, Wuq, k, 512 + 32 * h, I["w_mla_uq"], r0, 96 * h + 64, 32, gs=gq, gk=k, mul=QS)
                    load_w(stg, Wuq, k, 768 + 32 * h, I["w_mla_uq"], r0, 96 * h + 80, 16, gs=gq, gk=k, mul=-QS)
                    load_w(stg, Wuq, k, 768 + 32 * h + 16, I["w_mla_uq"], r0, 96 * h + 64, 16, gs=gq, gk=k, mul=QS)
            for h in range(8):
                load_w(stg, Wukv, 0, 64 * h, I["w_mla_ukv"], 0, 128 * h, 64, gs=gkv, gk=0)
                load_w(stg, Wukv, 0, 512 + 64 * h, I["w_mla_ukv"], 0, 128 * h + 64, 64, gs=gkv, gk=0)
            nst = make_norm_T(es, 8)
            hTs = [T(es, [128, 8, 512], BF16) for _ in range(2)]
            mcT = T(es, [128, 3, 512], BF16)
            pra = PS(es, [128, 512])
            prb = PS(es, [128, 512])
            pfm = [pra, prb]
            ptm = [PS(es, [128, 512]) for _ in range(2)]
            tpm = PS(es, [128, 3, 128], BF16)
            fst = [T(es, [128, 512], BF16) for _ in range(7)]
            vst = [T(es, [128, 16, 65], BF16) for _ in range(2)]
            for v in vst:
                op("pool", lambda e, v=v: e.memset(v.h[:], 1.0), [], [v])
            zerob = T(es, [8, 512], BF16)
            op("pool", lambda e: e.memset(zerob.h[:], 0.0), [], [zerob])
            for h in range(8):
                for c in range(NCH):
                    dma("pool", qa[h, 64:72, 512 * c:512 * c + 512], zerob.h[:, :], reads=[zerob])
                    dma("pool", ka[h, 64:72, 512 * c:512 * c + 512], zerob.h[:, :], reads=[zerob])
            cs4 = [T(es, [128, 512], F32) for _ in range(2)]
            sn4 = [T(es, [128, 512], F32) for _ in range(2)]
            t1 = [T(es, [128, 512], F32) for _ in range(2)]
            t2 = [T(es, [128, 512], F32) for _ in range(2)]
            sq = [T(es, [128, 1], F32) for _ in range(4)]
            rs = [T(es, [128, 1], F32) for _ in range(4)]
            xn = [T(es, [128, 384], BF16) for _ in range(2)]
            junk = nst["junk"]
            gi = 0
            ri = 0

            def evac(ps, st, M):
                nonlocal gi
                gi += 1
                if gi % 2 == 0:
                    op("act", lambda e: e.copy(out=st.h[:M, :], in_=ps.h[:M, :]), [ps], [st])
                else:
                    op("dve", lambda e: e.tensor_copy(out=st.h[:M, :], in_=ps.h[:M, :]), [ps], [st])

            def rope(pa, pb, M, cst, snt, st):
                nonlocal ri
                a, b = t1[ri % 2], t2[ri % 2]
                ri += 1
                op("dve", lambda e: e.tensor_tensor(out=a.h[:M, :], in0=pa.h[:M, :], in1=cst.h[:M, :], op=ALU.mult),
                   [pa, cst], [a])
                op("dve", lambda e: e.tensor_tensor(out=b.h[:M, :], in0=pb.h[:M, :], in1=snt.h[:M, :], op=ALU.mult),
                   [pb, snt], [b])
                op("pool", lambda e: e.tensor_tensor(out=st.h[:M, :], in0=a.h[:M, :], in1=b.h[:M, :], op=ALU.add),
                   [a, b], [st])

            def load_x(c):
                for b in range(4):
                    blk = 4 * c + b
                    xt = nst["xt"][blk % 8]
                    dma("sp", xt.h[:], xsrc[128 * blk:128 * blk + 128, :], writes=[xt])

            for c in range(NCH):
                hT = hTs[c % 2]
                cs = slice(512 * c, 512 * c + 512)
                cst, snt = cs4[c % 2], sn4[c % 2]
                dma("act", cst.h[:], I["c_cos4"][:, cs], writes=[cst])
                dma("act", snt.h[:], I["c_sin4"][:, cs], writes=[snt])
                if c == 0:
                    load_x(0)
                if c + 1 < NCH:
                    load_x(c + 1)
                for b in range(4):
                    blk = 4 * c + b
                    xt = nst["xt"][blk % 8]
                    norm_T(nst, xt, hT, b)
                for gidx in range(8):
                    ps, st = pfm[gidx % 2], fst[gidx % 4]
                    fm_group(W, 8, 128 * gidx, 128, hT, ps)
                    evac(ps, st, 128)
                    for half in range(2):
                        hd = 2 * gidx + half
                        dst = qa[hd, 0:64, cs] if hd < 8 else ka[hd - 8, 0:64, cs]
                        dma("sp", dst, st.h[64 * half:64 * half + 64, :], reads=[st])
                fm_group(W, 8, 1024, 32, hT, pra)
                fm_group(W, 8, 1056, 32, hT, prb)
                st = fst[6]
                rope(pra, prb, 32, cst, snt, st)
                for h in range(8):
                    dma("pool", ka[8 + h, 64:96, cs], st.h[0:32, :], reads=[st])
                for b in range(4):
                    blk = 4 * c + b
                    tok = slice(128 * blk, 128 * blk + 128)
                    vs = vst[blk % 2]
                    ps = ptm[0]
                    for k in range(8):
                        op("pe", lambda e, k=k: e.matmul(ps.h[:, :], hT.h[:, k, 128 * b:128 * b + 128],
                                                         W.h[:, k, 1088:1600], start=(k == 0), stop=(k == 7)),
                           [W, hT], [ps])
                    op("act", lambda e: e.copy(out=vs.h[:, 0:8, 0:64],
                                               in_=ps.h[:, :].rearrange("p (h d) -> p h d", d=64)), [ps], [vs],
                       partial=True)
                    ps = ptm[1]
                    for k in range(8):
                        op("pe", lambda e, k=k: e.matmul(ps.h[:, 0:384], hT.h[:, k, 128 * b:128 * b + 128],
                                                         W.h[:, k, 1600:1984], start=(k == 0), stop=(k == 7)),
                           [W, hT], [ps])
                    x_ = xn[blk % 2]
                    for pi, (a0, a1) in enumerate(((0, 256), (256, 384))):
                        ssq, rstd = sq[2 * (blk % 2) + pi], rs[2 * (blk % 2) + pi]
                        op("act", lambda e: e.activation(out=junk.h[:, a0:a1], in_=ps.h[:, a0:a1], func=AF.Square,
                                                         accum_out=ssq.h[:]), [ps], [junk, ssq])
                        rstd_of(ssq, rstd, a1 - a0)
                        op("dve", lambda e: e.tensor_scalar(out=x_.h[:, a0:a1], in0=ps.h[:, a0:a1], scalar1=rstd.h[:],
                                                            scalar2=None, op0=ALU.mult), [ps, rstd], [x_], partial=True)
                    for k in range(3):
                        op("pe", lambda e, k=k: e.transpose(out=tpm.h[:, k, :], in_=x_.h[:, 128 * k:128 * k + 128],
                                                            identity=identb.h[:]), [x_, identb], [tpm])
                    op("act", lambda e: e.copy(out=mcT.h[:, :, 128 * b:128 * b + 128], in_=tpm.h[:]), [tpm], [mcT],
                       partial=True)
                    ps = ptm[0]
                    op("pe", lambda e: e.matmul(ps.h[:, :], mcT.h[:, 2, 128 * b:128 * b + 128], Wukv.h[:, 0, 512:1024],
                                                start=True, stop=True), [mcT, Wukv], [ps])
                    op("dve", lambda e: e.tensor_copy(out=vs.h[:, 8:16, 0:64],
                                                      in_=ps.h[:, :].rearrange("p (h d) -> p h d", d=64)), [ps], [vs],
                       partial=True)
                    dma("pool", vv[tok, :, :], vs.h[:], reads=[vs])
                for gidx in range(4):
                    ps, st = pfm[gidx % 2], fst[4 + gidx % 2]
                    fm_group(Wuq, 2, 128 * gidx, 128, mcT, ps)
                    evac(ps, st, 128)
                    for half in range(2):
                        dma("sp", qa[8 + 2 * gidx + half, 0:64, cs], st.h[64 * half:64 * half + 64, :], reads=[st])
                for grp in range(2):
                    fm_group(Wuq, 2, 512 + 128 * grp, 128, mcT, pra)
                    fm_group(Wuq, 2, 768 + 128 * grp, 128, mcT, prb)
                    st = fst[6]
                    rope(pra, prb, 128, cst, snt, st)
                    for i in range(4):
                        dma("pool", qa[8 + 4 * grp + i, 64:96, cs], st.h[32 * i:32 * i + 32, :], reads=[st])
                for gidx in range(4):
                    ps, st = pfm[gidx % 2], fst[4 + gidx % 2]
                    fm_group(Wukv, 1, 128 * gidx, 128, mcT, ps, kbase=2)
                    evac(ps, st, 128)
                    for half in range(2):
                        dma("sp", ka[8 + 2 * gidx + half, 0:64, cs], st.h[64 * half:64 * half + 64, :], reads=[st])
            sch.barrier()
            sch.emit()

    def diff_final(lam_init):
        def alloc(es):
            st = {"rec": [T(es, [128, 4, 1], F32) for _ in range(2)],
                  "on": [T(es, [128, 4, 128], F32) for _ in range(2)],
                  "a": T(es, [128, 4, 128], F32), "junk": T(es, [128, 128], BF16),
                  "ssq": T(es, [128, 4], F32), "rstd": T(es, [128, 4], F32),
                  "stg": [T(es, [128, 4, 128], BF16) for _ in range(2)], "i": 0,
                  "nl": T(es, [128, 1], F32)}
            lv = [T(es, [128, 64], F32) for _ in range(4)]
            for t, nm in zip(lv, ("lambda_q1", "lambda_k1", "lambda_q2", "lambda_k2")):
                dma("sp", t.h[:], I[nm].partition_broadcast(128), writes=[t])
            pr = T(es, [128, 64], F32)
            e1 = T(es, [128, 1], F32)
            e2 = T(es, [128, 1], F32)
            for (a, b, d) in ((lv[0], lv[1], e1), (lv[2], lv[3], e2)):
                op("dve", lambda e: e.tensor_tensor(out=pr.h[:], in0=a.h[:], in1=b.h[:], op=ALU.mult), [a, b], [pr])
                op("dve", lambda e: e.reduce_sum(out=d.h[:], in_=pr.h[:], axis=AX.X), [pr], [d])
                op("act", lambda e: e.activation(out=d.h[:], in_=d.h[:], func=AF.Exp), [d], [d])
            nl = st["nl"]
            op("dve", lambda e: e.scalar_tensor_tensor(out=nl.h[:], in0=e2.h[:], scalar=-float(lam_init), in1=e1.h[:],
                                                       op0=ALU.add, op1=ALU.subtract), [e1, e2], [nl])
            return st

        def fn(st, m, c, tps, Tp):
            j = m["qi"] % 2
            h = m["qi"] // 2
            on = st["on"][j]
            for k, o in enumerate(tps):
                rec = st["rec"][k]
                for u in range(4):
                    op("pe", lambda e, u=u: e.transpose(out=Tp.h[:, u, 0:65], in_=o.h[0:65, 128 * u:128 * u + 128],
                                                        identity=identf.h[0:65, 0:65]), [o, identf], [Tp])
                op("dve", lambda e: e.reciprocal(out=rec.h[:], in_=Tp.h[:, :, 64:65]), [Tp], [rec])
                op("dve", lambda e: e.tensor_tensor(out=on.h[:, :, 64 * k:64 * k + 64], in0=Tp.h[:, :, 0:64],
                                                    in1=rec.h[:].to_broadcast([128, 4, 64]), op=ALU.mult),
                   [Tp, rec], [on], partial=True)
            if j == 0:
                return
            i = st["i"]
            st["i"] += 1
            a, stg, ssq, rstd, junk, nl = st["a"], st["stg"][i % 2], st["ssq"], st["rstd"], st["junk"], st["nl"]
            on0, on1 = st["on"]
            op("dve", lambda e: e.scalar_tensor_tensor(out=a.h[:], in0=on1.h[:], scalar=nl.h[:], in1=on0.h[:],
                                                       op0=ALU.mult, op1=ALU.add), [on0, on1, nl], [a])
            for u in range(4):
                op("act", lambda e, u=u: e.activation(out=junk.h[:], in_=a.h[:, u, :], func=AF.Square,
                                                      accum_out=ssq.h[:, u:u + 1]), [a], [junk, ssq], partial=True)
            op("act", lambda e: e.activation(out=rstd.h[:], in_=ssq.h[:], func=AF.Sqrt, bias=epsb.h[:], scale=1.0 / 128),
               [ssq, epsb], [rstd])
            op("dve", lambda e: e.reciprocal(out=rstd.h[:], in_=rstd.h[:]), [rstd], [rstd])
            op("dve", lambda e: e.tensor_tensor(out=stg.h[:], in0=a.h[:],
                                                in1=rstd.h[:].unsqueeze(2).to_broadcast([128, 4, 128]), op=ALU.mult),
               [a, rstd], [stg])
            dma("pool", mixd[512 * c:512 * c + 512, 128 * h:128 * h + 128].rearrange("(u p) d -> p u d", p=128),
                stg.h[:], reads=[stg])
        return {"alloc": alloc, "fn": fn}

    def phase_outproj(w_src, xsrc, xdst, rowscale=None):
        with ExitStack() as es:
            Wo = T(es, [128, 8, D], BF16)
            stg = [T(es, [128, 1024], F32) for _ in range(3)]
            for k in range(8):
                if rowscale is not None and rowscale[k] is not None:
                    load_w(stg, Wo, k, 0, w_src, 128 * k, 0, D, gs=rowscale[k][0], gk=0, mul=rowscale[k][1])
                else:
                    load_w(stg, Wo, k, 0, w_src, 128 * k, 0, D)
            mx = [T(es, [128, D], BF16) for _ in range(3)]
            xt = [T(es, [128, D], F32) for _ in range(3)]
            xo = [T(es, [128, D], F32) for _ in range(2)]
            tp = [PS(es, [128, 8, 128], BF16) for _ in range(2)]
            mT = [T(es, [128, 8, 128], BF16) for _ in range(2)]
            po = [PS(es, [128, 1024]) for _ in range(2)]
            for blk in range(NB):
                tok = slice(128 * blk, 128 * blk + 128)
                m_, x_, o_, t_, mt_, p_ = mx[blk % 3], xt[blk % 3], xo[blk % 2], tp[blk % 2], mT[blk % 2], po[blk % 2]
                dma("sp", m_.h[:], mixd[tok, :], writes=[m_])
                dma("act", x_.h[:], xsrc[tok, :], writes=[x_])
                for k in range(8):
                    op("pe", lambda e, k=k, t_=t_, m_=m_: e.transpose(out=t_.h[:, k, :], in_=m_.h[:, 128 * k:128 * k + 128],
                                                                      identity=identb.h[:]), [m_, identb], [t_])
                op("act", lambda e, mt_=mt_, t_=t_: e.copy(out=mt_.h[:], in_=t_.h[:]), [t_], [mt_])
                for hf in range(2):
                    for k in range(8):
                        op("pe", lambda e, k=k, hf=hf, p_=p_, mt_=mt_: e.matmul(
                            p_.h[:, 512 * hf:512 * hf + 512], mt_.h[:, k, :], Wo.h[:, k, 512 * hf:512 * hf + 512],
                            start=(k == 0), stop=(k == 7)), [mt_, Wo], [p_])
                op("dve", lambda e, o_=o_, p_=p_, x_=x_: e.tensor_tensor(out=o_.h[:], in0=p_.h[:], in1=x_.h[:], op=ALU.add),
                   [p_, x_], [o_])
                dma("pool", xdst[tok, :], o_.h[:], reads=[o_])
            sch.barrier()
            sch.emit()

    def phase_ffn(layer, xsrc, xdst, final_g=None):
        with ExitStack() as es:

            g = load_gain(es, I["norm_ffn_g"][layer], D)
            Wg = T(es, [128, 8, FFN], BF16)
            Wu = T(es, [128, 8, FFN], BF16)
            Wd = T(es, [128, 22, D], BF16)
            stg = [T(es, [128, 704], F32) for _ in range(2)]
            gres = [Res() for _ in range(4)]
            ures = [Res() for _ in range(4)]
            for cc in range(4):
                for k in range(8):
                    load_w(stg, Wg, k, 704 * cc, I["w_ffn_gate"][layer], 128 * k, 704 * cc, 704, gs=g, gk=k, res=gres[cc])
                    load_w(stg, Wu, k, 704 * cc, I["w_ffn_up"][layer], 128 * k, 704 * cc, 704, gs=g, gk=k, res=ures[cc])
            for f in range(22):
                load_w(stg, Wd, f, 0, I["w_ffn_down"][layer], 128 * f, 0, D)
            nst = make_norm_T(es)
            hT = T(es, [128, 8, 512], BF16)
            zT = T(es, [128, 22, 512], BF16)
            sg = [T(es, [128, 512], BF16) for _ in range(2)]
            pg = [PS(es, [128, 512]) for _ in range(2)]
            pu = [PS(es, [128, 512]) for _ in range(2)]
            pd = [PS(es, [128, 1024]) for _ in range(1)]
            xr = [T(es, [128, D], F32) for _ in range(2)]
            if final_g is not None:
                gf = T(es, [128, D], F32)
                dma("sp", gf.h[:], final_g.partition_broadcast(128), writes=[gf])
                fs = [T(es, [128, 1], F32) for _ in range(4)]
                fj = nst["junk"]
            for c in range(NCH):
                for b in range(4):
                    blk = 4 * c + b
                    xt = nst["xt"][blk % 3]
                    dma("sp", xt.h[:], xsrc[128 * blk:128 * blk + 128, :], writes=[xt])
                    norm_T(nst, xt, hT, b)
                for f in range(22):
                    g_, u_, s_ = pg[f % 2], pu[f % 2], sg[f % 2]
                    c_lo, c_hi = (128 * f) // 704, (128 * f + 127) // 704
                    if c_lo == c_hi:
                        fm_group(Wg, 8, 128 * f, 128, hT, g_, wres=gres[c_lo])
                        fm_group(Wu, 8, 128 * f, 128, hT, u_, wres=ures[c_lo])
                    else:
                        op("pe", lambda e: e.matmul(g_.h[:, :], Wg.h[:, 0, 128 * f:128 * f + 128], hT.h[:, 0, :],
                                                    start=True, stop=False), [gres[c_lo], gres[c_hi], hT], [g_])
                        for k in range(1, 8):
                            op("pe", lambda e, k=k: e.matmul(g_.h[:, :], Wg.h[:, k, 128 * f:128 * f + 128], hT.h[:, k, :],
                                                             start=False, stop=(k == 7)), [gres[c_lo], hT], [g_])
                        op("pe", lambda e: e.matmul(u_.h[:, :], Wu.h[:, 0, 128 * f:128 * f + 128], hT.h[:, 0, :],
                                                    start=True, stop=False), [ures[c_lo], ures[c_hi], hT], [u_])
                        for k in range(1, 8):
                            op("pe", lambda e, k=k: e.matmul(u_.h[:, :], Wu.h[:, k, 128 * f:128 * f + 128], hT.h[:, k, :],
                                                             start=False, stop=(k == 7)), [ures[c_lo], hT], [u_])
                    op("act", lambda e, s_=s_, g_=g_: e.activation(out=s_.h[:], in_=g_.h[:], func=AF.Silu), [g_], [s_])
                    op("dve", lambda e, f=f, s_=s_, u_=u_: e.tensor_tensor(out=zT.h[:, f, :], in0=u_.h[:], in1=s_.h[:],
                                                                         op=ALU.mult), [u_, s_], [zT], partial=True)
                for b in range(4):
                    blk = 4 * c + b
                    p_ = pd[0]
                    o_ = xr[blk % 2]
                    dma("act", o_.h[:], xsrc[128 * blk:128 * blk + 128, :], writes=[o_])
                    for hf in range(2):
                        for f in range(22):
                            op("pe", lambda e, f=f, hf=hf, b=b, p_=p_: e.matmul(
                                p_.h[:, 512 * hf:512 * hf + 512], zT.h[:, f, 128 * b:128 * b + 128],
                                Wd.h[:, f, 512 * hf:512 * hf + 512], start=(f == 0), stop=(f == 21)), [zT, Wd], [p_])
                    op("dve", lambda e, o_=o_, p_=p_: e.tensor_tensor(out=o_.h[:], in0=p_.h[:], in1=o_.h[:],
                                                                      op=ALU.add), [p_, o_], [o_])
                    if final_g is not None:
                        ssq, rstd = fs[2 * (blk % 2)], fs[2 * (blk % 2) + 1]
                        op("act", lambda e, o_=o_, ssq=ssq: e.activation(out=fj.h[:], in_=o_.h[:], func=AF.Square,
                                                                         accum_out=ssq.h[:]), [o_], [fj, ssq])
                        rstd_of(ssq, rstd, D)
                        op("dve", lambda e, o_=o_, rstd=rstd: e.scalar_tensor_tensor(
                            out=o_.h[:], in0=o_.h[:], scalar=rstd.h[:], in1=gf.h[:], op0=ALU.mult, op1=ALU.mult),
                           [o_, rstd, gf], [o_])
                    dma("pool", xdst[128 * blk:128 * blk + 128, :], o_.h[:], reads=[o_])
            sch.barrier()
            sch.emit()

    setup_consts()
    sf = std_final()
    if 0 in layers:
        phase_proj_even(I["x"])
        phase_index()
        fox_maps = [dict(qi=h, ki=h, vu=[h], strip="c", near=False, bias=None, col=64 * h) for h in range(8)]
        phase_attn(fox_maps, 70, 2, sf)
        dsa_maps = [dict(qi=8 + h, ki=8 + h, vu=[8 + h], strip=h, near=True, bias=b31.h[:, h:h + 1],
                         col=512 + 64 * h) for h in range(8)]
        phase_attn(dsa_maps, 72, 4, sf, use_mneg=True, t5=True, prefetch=False)
        phase_outproj(I["w_out_even"], I["x"], xa)
        phase_ffn(0, xa, xb if 1 in layers else y_out, final_g=None if 1 in layers else I["final_norm_g"][0])
    if 1 in layers:
        LI = 0.8 - 0.6 * math.exp(-0.3 * 1)
        xin1 = xb if 0 in layers else I["x"]
        phase_proj_odd(xin1)
        diff_maps = [dict(qi=m, ki=m, vu=[2 * (m // 2), 2 * (m // 2) + 1], strip=m, near=True, bias=b31.h[:, m:m + 1])
                     for m in range(8)]
        phase_attn(diff_maps, 72, 2, diff_final(LI), nunits=2, t5=True)
        mla_maps = [dict(qi=8 + h, ki=8 + h, vu=[8 + h], strip="c", near=False, bias=None, col=512 + 64 * h)
                    for h in range(8)]
        phase_attn(mla_maps, 96, 2, sf)
        with ExitStack() as es2:
            sg_ = T(es2, [128, 1], F32)
            dma("sp", sg_.h[:], I["diff_subln_g"].rearrange("o d -> d o"), writes=[sg_], allow_slow_non_contiguous=True)
            rsc = [(sg_, 1.0 - LI)] * 4 + [None] * 4
            phase_outproj(I["w_out_odd"], xin1, xa, rowscale=rsc)
        phase_ffn(1, xa, y_out, final_g=I["final_norm_g"][0])
    sch.barrier()
    top.close()
    return nc


_CACHE = {}


def _host_inputs(inputs, S, b):
    consts, bucket = _consts(S)
    t5 = np.asarray(inputs["t5_bias"], dtype=np.float32)
    m = {"x": np.ascontiguousarray(np.asarray(inputs["x"])[b])}
    for k in ("norm_mix_g", "norm_ffn_g", "w_ffn_gate", "w_ffn_up", "w_ffn_down"):
        m[k] = np.ascontiguousarray(np.asarray(inputs[k], dtype=np.float32))
    for k in ("w_in_even", "w_out_even", "w_in_odd", "w_mla_uq", "w_mla_ukv", "w_out_odd"):
        m[k] = np.ascontiguousarray(np.asarray(inputs[k], dtype=np.float32)[0])
    for k in ("b_forget", "lambda_q1", "lambda_k1", "lambda_q2", "lambda_k2", "diff_subln_g", "mla_q_norm_g",
              "mla_kv_norm_g"):
        m[k] = np.ascontiguousarray(np.asarray(inputs[k], dtype=np.float32).reshape(1, -1))
    m["final_norm_g"] = np.ascontiguousarray(np.asarray(inputs["final_norm_g"], dtype=np.float32).reshape(1, -1))
    m["t5_31"] = np.ascontiguousarray(t5[31:32, :])
    m["t5strip"] = np.ascontiguousarray(np.transpose(t5[bucket], (2, 0, 1)))
    m.update(consts)
    return m


def kernel(**inputs):
    x = np.asarray(inputs["x"])
    B, S, _ = x.shape
    key = (S,)
    if key not in _CACHE:
        _CACHE[key] = build(S)
    nc = _CACHE[key]
    in_maps = [_host_inputs(inputs, S, b) for b in range(B)]
    res = run_bass_kernel_spmd(nc, in_maps, core_ids=list(range(B)))
    return np.stack([np.asarray(r["y"], dtype=np.float32) for r in res.results], axis=0)
```

```python
import math
from contextlib import ExitStack

import numpy as np
import concourse.bass as bass
import concourse.mybir as mybir
from concourse.bass_utils import run_bass_kernel_spmd

F32 = mybir.dt.float32
BF16 = mybir.dt.bfloat16
AF = mybir.ActivationFunctionType
ALU = mybir.AluOpType
AX = mybir.AxisListType

D = 1024
FFN = 2816
NEG = -30000.0
EPS = 1e-6
TOPK = 256
NIT = 16


class Res:
    __slots__ = ("w", "r")

    def __init__(self):
        self.w = {}
        self.r = {}


class TB:
    def __init__(self, h):
        self.h = h
        self.res = Res()


class Rec:
    def __init__(self):
        self.rec = None

    def __getattr__(self, name):
        def f(*a, **k):
            self.rec = (name, a, k)
            return self
        return f


class EngQ:
    def __init__(self, name, semid):
        self.name = name
        self.semid = semid
        self.cnt = 0
        self.ops = []
        self.waited = {}


class Sched:
    def __init__(self, nc, es):
        self.nc = nc
        self.sems = []
        self.engs = {}
        for name in ("pe", "act", "dve", "pool", "sp"):
            self.sems.append(es.enter_context(nc.semaphore("s_" + name)))
            self.engs[name] = EngQ(name, len(self.sems) - 1)
        self.dq = {}
        for qn, k in (("sp", 12), ("act", 6), ("pool", 8)):
            lst = []
            for i in range(k):
                self.sems.append(es.enter_context(nc.semaphore("d_%s%d" % (qn, i))))
                lst.append([len(self.sems) - 1, 0])
            self.dq[qn] = [lst, 0]

    def _wait(self, q, deps):
        for s, v in deps.items():
            if q.waited.get(s, 0) < v:
                q.ops.append(("w", s, v))
                q.waited[s] = v

    @staticmethod
    def _merge(d, s):
        for k, v in s.items():
            if d.get(k, 0) < v:
                d[k] = v

    def _deps(self, reads, writes, partial):
        deps = {}
        for R in reads:
            self._merge(deps, R.w)
        for R in writes:
            self._merge(deps, R.r)
            if not partial:
                self._merge(deps, R.w)
        return deps

    def _record(self, ev, reads, writes, partial):
        for R in writes:
            if partial:
                self._merge(R.w, ev)
            else:
                R.w = dict(ev)
                R.r = {}
        for R in reads:
            self._merge(R.r, ev)

    def op(self, eng, fn, reads=(), writes=(), partial=False):
        q = self.engs[eng]
        reads = [r.res if isinstance(r, TB) else r for r in reads]
        writes = [r.res if isinstance(r, TB) else r for r in writes]
        deps = self._deps(reads, writes, partial)
        if eng == "pe":
            deps.pop(q.semid, None)
        self._wait(q, deps)
        q.cnt += 1
        r = Rec()
        fn(r)
        q.ops.append(("i", r.rec))
        self._record({q.semid: q.cnt}, reads, writes, partial)

    def dma(self, queue, out, in_, reads=(), writes=(), partial=False, **kw):
        q = self.engs[queue]
        reads = [r.res if isinstance(r, TB) else r for r in reads]
        writes = [r.res if isinstance(r, TB) else r for r in writes]
        deps = self._deps(reads, writes, partial)
        lst, idx = self.dq[queue]
        slot = lst[idx]
        self.dq[queue][1] = (idx + 1) % len(lst)
        if slot[1] > 0:
            deps[slot[0]] = max(deps.get(slot[0], 0), slot[1])
        self._wait(q, deps)
        slot[1] += 16
        q.ops.append(("d", (lambda e, o=out, i=in_, k=kw: e.dma_start(out=o, in_=i, **k)), slot[0]))
        self._record({slot[0]: slot[1]}, reads, writes, partial)

    def barrier(self):
        tot = {}
        for q in self.engs.values():
            tot[q.semid] = q.cnt
        for lst, _ in self.dq.values():
            for s, v in lst:
                if v:
                    tot[s] = v
        for q in self.engs.values():
            d = {s: v for s, v in tot.items() if v > 0 and s != q.semid}
            self._wait(q, d)

    def emit(self):
        nc = self.nc
        sems = self.sems

        def run(q, e):
            own = sems[q.semid]
            for o in q.ops:
                if o[0] == "w":
                    e.wait_ge(sems[o[1]], o[2])
                elif o[0] == "i":
                    name, a, k = o[1]
                    getattr(e, name)(*a, **k).then_inc(own, 1)
                else:
                    o[1](e).then_inc(sems[o[2]], 16)
            q.ops = []

        with nc.Block() as block:
            @block.sync
            def _(e):
                run(self.engs["sp"], e)

            @block.tensor
            def _(e):
                run(self.engs["pe"], e)

            @block.scalar
            def _(e):
                run(self.engs["act"], e)

            @block.vector
            def _(e):
                run(self.engs["dve"], e)

            @block.gpsimd
            def _(e):
                run(self.engs["pool"], e)


def _t5_bucket_np(d):
    d = np.asarray(d)
    exact = 16
    df = np.maximum(d, 1).astype(np.float32)
    lb = exact + (np.log(df / np.float32(exact)) / np.float32(math.log(128 / exact))
                  * np.float32(32 - exact)).astype(np.int32)
    lb = np.minimum(lb, 31)
    return np.where(d < exact, d, lb)


def _consts(S):
    c = {}
    idx = np.arange(128)
    c["c_ident"] = np.eye(128, dtype=np.float32)
    c["c_ntriu"] = -(idx[:, None] <= idx[None, :]).astype(np.float32)
    col = np.arange(640)
    dd = col[None, :] - idx[:, None]
    c["c_cmstrip"] = np.where(dd >= 0, 0.0, NEG).astype(np.float32)
    c["c_cmtok"] = np.where(idx[None, :] <= idx[:, None], 0.0, NEG).astype(np.float32)
    pos = np.arange(S, dtype=np.float32)
    inv = (np.float32(10000.0) ** (-np.arange(0, 32, 2, dtype=np.float32) / np.float32(32))).astype(np.float32)
    ang = (pos[:, None] * inv[None, :]).astype(np.float32)
    ang = np.concatenate([ang, ang], axis=-1)
    cos = np.cos(ang).astype(np.float32).T
    sin = np.sin(ang).astype(np.float32).T
    c["c_cos4"] = np.ascontiguousarray(np.tile(cos, (4, 1)))
    c["c_sin4"] = np.ascontiguousarray(np.tile(sin, (4, 1)))
    bucket = _t5_bucket_np(np.maximum(dd, 0))
    return c, bucket


def build(S, layers=(0, 1), dbg=False):
    NB = S // 128
    NCH = S // 512
    nc = bass.Bass("TRN2", target_bir_lowering=False)

    def din(name, shape):
        return nc.dram_tensor(name, list(shape), F32, kind="ExternalInput").ap()

    def dscr(name, shape, dt=BF16):
        if dbg:
            return nc.dram_tensor(name, list(shape), dt, kind="ExternalOutput").ap()
        return nc.dram_tensor(name, list(shape), dt).ap()

    I = {}
    for name, shape in [
        ("x", [S, D]), ("norm_mix_g", [2, D]), ("norm_ffn_g", [2, D]),
        ("w_in_even", [D, 3404]), ("b_forget", [1, 8]), ("w_out_even", [D, D]),
        ("w_in_odd", [D, 1952]), ("lambda_q1", [1, 64]), ("lambda_k1", [1, 64]),
        ("lambda_q2", [1, 64]), ("lambda_k2", [1, 64]), ("diff_subln_g", [1, 128]),
        ("mla_q_norm_g", [1, 256]), ("w_mla_uq", [256, 768]), ("mla_kv_norm_g", [1, 128]),
        ("w_mla_ukv", [128, 1024]), ("w_out_odd", [D, D]), ("t5_31", [1, 8]),
        ("t5strip", [8, 128, 640]),
        ("w_ffn_gate", [2, D, FFN]), ("w_ffn_up", [2, D, FFN]), ("w_ffn_down", [2, FFN, D]),
        ("final_norm_g", [1, D]),
        ("c_ident", [128, 128]), ("c_ntriu", [128, 128]), ("c_cmstrip", [128, 640]),
        ("c_cmtok", [128, 128]), ("c_cos4", [128, S]), ("c_sin4", [128, S]),
    ]:
        I[name] = din(name, shape)
    y_out = nc.dram_tensor("y", [S, D], F32, kind="ExternalOutput").ap()

    qa = dscr("qa", [16, 96, S])
    ka = dscr("ka", [16, 96, S])
    vv = dscr("vv", [S, 16, 65])
    iqd = dscr("iqd", [4, 64, S])
    ikd = dscr("ikd", [64, S])
    mnegd = dscr("mnegd", [S, S])
    mixd = dscr("mixd", [S, D])
    xa = dscr("xa", [S, D], F32)
    xb = dscr("xb", [S, D], F32)

    top = ExitStack()
    sch = Sched(nc, top)
    op, dma = sch.op, sch.dma
    cnt = [0]

    def T(es, shape, dt):
        cnt[0] += 1
        return TB(es.enter_context(nc.sbuf_tensor("t%d" % cnt[0], list(shape), dt)))

    def PS(es, shape, dt=F32):
        cnt[0] += 1
        return TB(es.enter_context(nc.psum_tensor("p%d" % cnt[0], list(shape), dt)))

    identf = T(top, [128, 128], F32)
    identb = T(top, [128, 128], BF16)
    ntriu = T(top, [128, 128], F32)
    onesf = T(top, [128, 128], F32)
    cmtok = T(top, [128, 128], F32)
    cstrip = T(top, [128, 128], BF16)
    b31 = T(top, [128, 8], F32)
    iwabs = T(top, [128, NB, 4], F32)
    isgn = T(top, [128, NB, 4], F32)
    epsb = T(top, [128, 1], F32)

    def setup_consts():
        with ExitStack() as es:
            tmp = T(es, [128, 640], F32)
            dma("sp", identf.h[:], I["c_ident"], writes=[identf])
            dma("sp", ntriu.h[:], I["c_ntriu"], writes=[ntriu])
            dma("sp", cmtok.h[:], I["c_cmtok"], writes=[cmtok])
            dma("sp", b31.h[:], I["t5_31"].partition_broadcast(128), writes=[b31])
            dma("sp", tmp.h[:], I["c_cmstrip"], writes=[tmp])
            op("dve", lambda e: e.tensor_copy(out=identb.h[:], in_=identf.h[:]), [identf], [identb])
            op("dve", lambda e: e.memset(onesf.h[:], 1.0), [], [onesf])
            op("dve", lambda e: e.memset(epsb.h[:], EPS), [], [epsb])
            op("dve", lambda e: e.tensor_copy(out=cstrip.h[:], in_=tmp.h[:, 0:128]), [tmp], [cstrip])
            sch.barrier()
            sch.emit()

    def load_w(stg, dst, dst_k, dst_c0, src, r0, c0, cw, gs=None, gk=0, mul=None, ctr=[0], res=None):
        CW = stg[0].h.shape[1]
        for o in range(0, cw, CW):
            w = min(CW, cw - o)
            st = stg[ctr[0] % len(stg)]
            eng = "dve"
            ctr[0] += 1
            dma("sp", st.h[:, :w], src[r0:r0 + 128, c0 + o:c0 + o + w], writes=[st])
            dv = dst.h[:, dst_k, dst_c0 + o:dst_c0 + o + w]
            if gs is not None:
                rd = [st, gs]
                if mul is None:
                    f = lambda e, dv=dv, st=st, w=w: e.tensor_scalar(
                        out=dv, in0=st.h[:, :w], scalar1=gs.h[:, gk:gk + 1], scalar2=None, op0=ALU.mult)
                else:
                    f = lambda e, dv=dv, st=st, w=w: e.tensor_scalar(
                        out=dv, in0=st.h[:, :w], scalar1=gs.h[:, gk:gk + 1], scalar2=float(mul),
                        op0=ALU.mult, op1=ALU.mult)
            else:
                rd = [st]
                if mul is None:
                    f = lambda e, dv=dv, st=st, w=w: e.tensor_copy(out=dv, in_=st.h[:, :w])
                else:
                    f = lambda e, dv=dv, st=st, w=w: e.tensor_scalar(
                        out=dv, in0=st.h[:, :w], scalar1=float(mul), scalar2=None, op0=ALU.mult)
            op(eng, f, rd, [dst if res is None else res], partial=True)

    def load_gain(es, src_row, n):
        k = n // 128
        g = T(es, [128, k], F32)
        dma("sp", g.h[:], src_row.rearrange("(k p) -> p k", p=128), writes=[g],
            allow_slow_non_contiguous=True)
        return g

    def make_norm_T(es, nxt=3):
        st = {
            "xt": [T(es, [128, D], F32) for _ in range(nxt)],
            "junk": T(es, [128, D], BF16),
            "ssq": [T(es, [128, 1], F32) for _ in range(3)],
            "rstd": [T(es, [128, 1], F32) for _ in range(3)],
            "xn": [T(es, [128, D], BF16) for _ in range(2)],
            "tp": [PS(es, [128, 8, 128], BF16) for _ in range(2)],
            "i": 0,
        }
        return st

    def rstd_of(ssq, rstd, n, junk_unused=None):
        op("act", lambda e: e.activation(out=rstd.h[:], in_=ssq.h[:], func=AF.Sqrt, bias=epsb.h[:],
                                         scale=1.0 / n), [ssq, epsb], [rstd])
        op("dve", lambda e: e.reciprocal(out=rstd.h[:], in_=rstd.h[:]), [rstd], [rstd])

    def norm_T(st, xt, hT, b):
        i = st["i"]
        st["i"] += 1
        ssq, rstd, xn, tp = st["ssq"][i % 3], st["rstd"][i % 3], st["xn"][i % 2], st["tp"][i % 2]
        junk = st["junk"]
        op("act", lambda e: e.activation(out=junk.h[:], in_=xt.h[:], func=AF.Square, accum_out=ssq.h[:]),
           [xt], [junk, ssq])
        rstd_of(ssq, rstd, D)
        op("dve", lambda e: e.tensor_scalar(out=xn.h[:], in0=xt.h[:], scalar1=rstd.h[:], scalar2=None,
                                            op0=ALU.mult), [xt, rstd], [xn])
        for k in range(8):
            op("pe", lambda e, k=k: e.transpose(out=tp.h[:, k, :], in_=xn.h[:, 128 * k:128 * k + 128],
                                                identity=identb.h[:]), [xn, identb], [tp])
        op("act", lambda e: e.copy(out=hT.h[:, :, 128 * b:128 * b + 128], in_=tp.h[:]), [tp], [hT],
           partial=True)

    def fm_group(W, KC, col0, M, hT, ps, kbase=0, wres=None):
        for k in range(KC):
            op("pe", lambda e, k=k: e.matmul(ps.h[:M, :], W.h[:, k, col0:col0 + M], hT.h[:, kbase + k, :],
                                             start=(k == 0), stop=(k == KC - 1)), [W if wres is None else wres, hT], [ps])

    def phase_proj_even(xsrc):
        with ExitStack() as es:
            g = load_gain(es, I["norm_mix_g"][0], D)
            W = T(es, [128, 8, 3404], BF16)
            stg = [T(es, [128, 1702], F32) for _ in range(3)]
            cmap = [(0, 0, 512, 0.125), (512, 512, 512, None), (1544, 1024, 512, 0.125), (2056, 1536, 512, None),
                    (3080, 2048, 256, 0.125 * 0.5), (3336, 2304, 64, None),
                    (1024, 2368, 512, None), (2568, 2880, 512, None), (1536, 3392, 8, None), (3400, 3400, 4, None)]
            wres = [Res() for _ in cmap]
            bounds = [dc for (_, dc, _, _) in cmap]

            def went(col):
                e = 0
                for i_, d_ in enumerate(bounds):
                    if col >= d_:
                        e = i_
                return wres[e]
            for ei, (sc, dc, cw, mul) in enumerate(cmap):
                for k in range(8):
                    load_w(stg, W, k, dc, I["w_in_even"], 128 * k, sc, cw, gs=g, gk=k, mul=mul, res=wres[ei])
            nst = make_norm_T(es, 8)
            hTs = [T(es, [128, 8, 512], BF16) for _ in range(2)]
            pfm = [PS(es, [128, 512]) for _ in range(2)]
            ptm = [PS(es, [128, 512]) for _ in range(2)]
            psm = PS(es, [128, 512])
            pct = PS(es, [128, 128], BF16)
            fst = [T(es, [128, 512], BF16) for _ in range(6)]
            vst = [T(es, [128, 16, 65], BF16) for _ in range(2)]
            bfb = T(es, [128, 8], F32)
            dma("sp", bfb.h[:], I["b_forget"].partition_broadcast(128), writes=[bfb])
            carry = T(es, [1, 8], F32)
            op("dve", lambda e: e.memset(carry.h[:], 0.0), [], [carry])
            for v in vst:
                op("pool", lambda e, v=v: e.memset(v.h[:], 1.0), [], [v])
            onesb = T(es, [128, 512], BF16)
            op("pool", lambda e: e.memset(onesb.h[:], 1.0), [], [onesb])
            for h in range(8):
                for c in range(NCH):
                    dma("pool", qa[h, 67:70, 512 * c:512 * c + 512], onesb.h[0:3, :], reads=[onesb])
                    dma("pool", ka[h, 64:67, 512 * c:512 * c + 512], onesb.h[0:3, :], reads=[onesb])
            zerob = T(es, [8, 512], BF16)
            op("pool", lambda e: e.memset(zerob.h[:], 0.0), [], [zerob])
            for h in range(8, 16):
                for c in range(NCH):
                    dma("pool", qa[h, 64:72, 512 * c:512 * c + 512], zerob.h[:, :], reads=[zerob])
                    dma("pool", ka[h, 64:72, 512 * c:512 * c + 512], zerob.h[:, :], reads=[zerob])
            xb8 = T(es, [128, 8], F32)
            ee = T(es, [128, 8], F32)
            lls = [T(es, [128, 8], F32) for _ in range(2)]
            c32 = T(es, [128, 8], F32)
            t32 = T(es, [128, 8], F32)
            r1 = T(es, [128, 8], F32)
            CT = T(es, [128, 8, 6], BF16)
            CTs = [T(es, [48, 512], BF16) for _ in range(2)]
            tmp4 = T(es, [128, 4], F32)
            gi = 0
            def early(c, b, blk):
                hT = hTs[c % 2]
                ll = lls[blk % 2]
                for k in range(8):
                    op("pe", lambda e, k=k: e.matmul(psm.h[:, 0:12], hT.h[:, k, 128 * b:128 * b + 128],
                                                     W.h[:, k, 3392:3404], start=(k == 0), stop=(k == 7)),
                       [wres[8], wres[9], hT], [psm])
                op("dve", lambda e: e.tensor_scalar(out=tmp4.h[:], in0=psm.h[:, 8:12], scalar1=0.0, scalar2=2.0,
                                                    op0=ALU.is_ge, op1=ALU.mult), [psm], [tmp4])
                op("dve", lambda e: e.tensor_scalar_add(out=isgn.h[:, blk, :], in0=tmp4.h[:], scalar1=-1.0),
                   [tmp4], [isgn], partial=True)
                op("dve", lambda e: e.tensor_tensor(out=iwabs.h[:, blk, :], in0=psm.h[:, 8:12],
                                                    in1=isgn.h[:, blk, :], op=ALU.mult), [psm, isgn], [iwabs], partial=True)
                op("dve", lambda e: e.tensor_tensor(out=xb8.h[:], in0=psm.h[:, 0:8], in1=bfb.h[:], op=ALU.add),
                   [psm, bfb], [xb8])
                op("act", lambda e: e.activation(out=ee.h[:], in_=xb8.h[:], func=AF.Exp, scale=-1.0), [xb8], [ee])
                op("act", lambda e: e.activation(out=ll.h[:], in_=ee.h[:], func=AF.Ln, bias=1.0), [ee], [ll])

            def late(c, b, blk, cts):
                ll = lls[blk % 2]
                op("pe", lambda e: e.matmul(psm.h[:, 16:24], ntriu.h[:], ll.h[:], start=True, stop=False),
                   [ntriu, ll], [psm], partial=True)
                op("pe", lambda e: e.matmul(psm.h[:, 16:24], onesf.h[0:1, :], carry.h[0:1, :], start=False, stop=True),
                   [onesf, carry], [psm], partial=True)
                op("pe", lambda e: e.matmul(psm.h[0:1, 32:40], onesf.h[:, 0:1], ll.h[:], start=True, stop=True),
                   [onesf, ll], [psm], partial=True)
                op("dve", lambda e: e.tensor_copy(out=c32.h[:], in_=psm.h[:, 16:24]), [psm], [c32])
                op("dve", lambda e: e.tensor_tensor(out=carry.h[:], in0=carry.h[:], in1=psm.h[0:1, 32:40],
                                                    op=ALU.subtract), [carry, psm], [carry])
                op("dve", lambda e: e.tensor_copy(out=CT.h[:, :, 0], in_=c32.h[:]), [c32], [CT], partial=True)
                op("dve", lambda e: e.tensor_copy(out=t32.h[:], in_=CT.h[:, :, 0]), [CT], [t32])
                op("dve", lambda e: e.tensor_tensor(out=r1.h[:], in0=c32.h[:], in1=t32.h[:], op=ALU.subtract),
                   [c32, t32], [r1])
                op("dve", lambda e: e.tensor_copy(out=CT.h[:, :, 1], in_=r1.h[:]), [r1], [CT], partial=True)
                op("dve", lambda e: e.tensor_copy(out=t32.h[:], in_=CT.h[:, :, 1]), [CT], [t32])
                op("dve", lambda e: e.tensor_tensor(out=r1.h[:], in0=r1.h[:], in1=t32.h[:], op=ALU.subtract),
                   [r1, t32], [r1])
                op("dve", lambda e: e.tensor_copy(out=CT.h[:, :, 2], in_=r1.h[:]), [r1], [CT], partial=True)
                op("dve", lambda e: e.tensor_scalar(out=CT.h[:, :, 3:6], in0=CT.h[:, :, 0:3], scalar1=-1.0,
                                                    scalar2=None, op0=ALU.mult), [CT], [CT], partial=True)
                op("pe", lambda e: e.transpose(out=pct.h[0:48, :], in_=CT.h[:].rearrange("p h s -> p (h s)"),
                                               identity=identb.h[:]), [CT, identb], [pct])
                op("dve", lambda e: e.tensor_copy(out=cts.h[:, 128 * b:128 * b + 128], in_=pct.h[0:48, :]),
                   [pct], [cts], partial=True)

            pend_late = None
            def load_x(c):
                for b in range(4):
                    blk = 4 * c + b
                    xt = nst["xt"][blk % 8]
                    dma("sp", xt.h[:], xsrc[128 * blk:128 * blk + 128, :], writes=[xt])
            load_x(0)
            for c in range(NCH):
                hT = hTs[c % 2]
                cts = CTs[c % 2]
                if c + 1 < NCH:
                    load_x(c + 1)
                for b in range(4):
                    blk = 4 * c + b
                    xt = nst["xt"][blk % 8]
                    norm_T(nst, xt, hT, b)
                for gidx in range(19):
                    col0 = 128 * gidx
                    M = min(128, 2368 - col0)
                    ps = pfm[gi % 2]
                    st = fst[gi % 6]
                    gi += 1
                    fm_group(W, 8, col0, M, hT, ps, wres=went(col0))
                    if gi % 2 == 0:
                        op("act", lambda e, st=st, ps=ps, M=M: e.copy(out=st.h[:M, :], in_=ps.h[:M, :]), [ps], [st])
                    else:
                        op("dve", lambda e, st=st, ps=ps, M=M: e.tensor_copy(out=st.h[:M, :], in_=ps.h[:M, :]),
                           [ps], [st])
                    cs = slice(512 * c, 512 * c + 512)
                    for half in range(M // 64):
                        hd = 2 * gidx + half
                        src = st.h[64 * half:64 * half + 64, :]
                        if hd < 8:
                            dst = qa[hd, 0:64, cs]
                        elif hd < 16:
                            dst = ka[hd - 8, 0:64, cs]
                        elif hd < 24:
                            dst = qa[hd - 8, 0:64, cs]
                        elif hd < 32:
                            dst = ka[hd - 16, 0:64, cs]
                        elif hd < 36:
                            dst = iqd[hd - 32, :, cs]
                        else:
                            dst = ikd[:, cs]
                        dma("sp", dst, src, reads=[st])
                for b in range(4):
                    blk = 4 * c + b
                    tok = slice(128 * blk, 128 * blk + 128)
                    vs = vst[blk % 2]
                    for vi in range(2):
                        ps = ptm[vi]
                        for k in range(8):
                            op("pe", lambda e, k=k, ps=ps, vi=vi: e.matmul(
                                ps.h[:, :], hT.h[:, k, 128 * b:128 * b + 128],
                                W.h[:, k, 2368 + 512 * vi:2368 + 512 * vi + 512],
                                start=(k == 0), stop=(k == 7)), [went(2368 + 512 * vi), hT], [ps])
                        eng = "act" if vi == 0 else "dve"
                        if eng == "act":
                            op("act", lambda e, ps=ps, vs=vs, vi=vi: e.copy(
                                out=vs.h[:, 8 * vi:8 * vi + 8, 0:64],
                                in_=ps.h[:, :].rearrange("p (h d) -> p h d", d=64)), [ps], [vs], partial=True)
                        else:
                            op("dve", lambda e, ps=ps, vs=vs, vi=vi: e.tensor_copy(
                                out=vs.h[:, 8 * vi:8 * vi + 8, 0:64],
                                in_=ps.h[:, :].rearrange("p (h d) -> p h d", d=64)), [ps], [vs], partial=True)
                    dma("pool", vv[tok, :, :], vs.h[:], reads=[vs])
                    early(c, b, blk)
                    if pend_late is not None:
                        late(*pend_late)
                    pend_late = (c, b, blk, cts)
                late(*pend_late)
                pend_late = None
                cs = slice(512 * c, 512 * c + 512)
                for h in range(8):
                    dma("pool", qa[h, 64:67, cs], cts.h[6 * h:6 * h + 3, :], reads=[cts])
                    dma("pool", ka[h, 67:70, cs], cts.h[6 * h + 3:6 * h + 6, :], reads=[cts])
            sch.barrier()
            sch.emit()

    def phase_index():
        with ExitStack() as es:
            iq = T(es, [128, 2, S], BF16)
            ik = T(es, [128, S], BF16)
            for h in range(4):
                dma("sp", iq.h[64 * (h % 2):64 * (h % 2) + 64, h // 2, :], iqd[h], writes=[iq], partial=True)
            for r in range(2):
                dma("sp", ik.h[64 * r:64 * r + 64, :], ikd, writes=[ik], partial=True)
            SCs = [T(es, [128, S], F32) for _ in range(2)]
            junks = [T(es, [128, S], BF16) for _ in range(2)]
            MNs = [T(es, [128, S], BF16) for _ in range(2)]
            Rt = [T(es, [128, 512], BF16) for _ in range(8)]
            Dg = [T(es, [128, 128], BF16) for _ in range(8)]
            X = [PS(es, [128, 512]) for _ in range(4)]
            SP_ = [PS(es, [128, 512]) for _ in range(2)]
            TPm = [PS(es, [128, 8, 128], BF16) for _ in range(2)]
            tst = [T(es, [128, 8, 128], BF16) for _ in range(3)]
            pw = T(es, [128, NIT + 1], F32)
            for it in range(NIT + 1):
                op("pool", lambda e, it=it: e.memset(pw.h[:, it:it + 1], float(2.0 ** (-(it + 1)))), [], [pw],
                   partial=True)
            sm = [[T(es, [128, 1], F32) for _ in range(8)] for _ in range(2)]
            stp = [T(es, [128, NIT + 1], F32) for _ in range(2)]
            nstp = [T(es, [128, NIT + 1], F32) for _ in range(2)]
            cn = {"ri": 0, "si": 0, "ti": 0, "tp": 0}

            def scores(i, SC):
                n = 128 * (i + 1)
                q0 = 128 * i
                dset = Dg[4 * (i % 2):4 * (i % 2) + 4]
                for h in range(4):
                    op("dve", lambda e, h=h: e.tensor_scalar(
                        out=dset[h].h[:], in0=identb.h[:], scalar1=isgn.h[:, i, h:h + 1], scalar2=None, op0=ALU.mult),
                       [identb, isgn], [dset[h]])
                nk = (n + 511) // 512
                pend = None
                for kc in range(nk + 1):
                    cur = None
                    if kc < nk:
                        k0 = 512 * kc
                        kw = min(512, n - k0)
                        rs = []
                        for h in range(4):
                            pb = 64 * (h % 2)
                            op("pe", lambda e, h=h, pb=pb: e.matmul(
                                X[h].h[:, :kw], iq.h[pb:pb + 64, h // 2, q0:q0 + 128], ik.h[pb:pb + 64, k0:k0 + kw],
                                start=True, stop=True), [iq, ik], [X[h]])
                            r = Rt[cn["ri"] % 8]
                            cn["ri"] += 1
                            rs.append(r)
                            if h < 2:
                                op("act", lambda e, h=h, r=r: e.activation(
                                    out=r.h[:, :kw], in_=X[h].h[:, :kw], func=AF.Relu, scale=iwabs.h[:, i, h:h + 1]),
                                   [X[h], iwabs], [r])
                            else:
                                op("dve", lambda e, h=h, r=r: e.tensor_scalar(
                                    out=r.h[:, :kw], in0=X[h].h[:, :kw], scalar1=iwabs.h[:, i, h:h + 1], scalar2=0.0,
                                    op0=ALU.mult, op1=ALU.max), [X[h], iwabs], [r])
                        cur = (k0, kw, rs)
                    if pend is not None:
                        pk0, pkw, prs = pend
                        sp = SP_[cn["si"] % 2]
                        cn["si"] += 1
                        for h in range(4):
                            op("pe", lambda e, h=h: e.matmul(
                                sp.h[:, :pkw], dset[h].h[:], prs[h].h[:, :pkw], start=(h == 0), stop=(h == 3)),
                               [dset[h], prs[h]], [sp])
                        op("act", lambda e: e.copy(out=SC.h[:, pk0:pk0 + pkw], in_=sp.h[:, :pkw]), [sp], [SC],
                           partial=True)
                    pend = cur

            def prep(i, SC, st, ch):
                n = 128 * (i + 1)
                A, lo, mid, cntt, ge, u2, tq, _ = sm[ch]
                op("dve", lambda e: e.tensor_reduce(out=A.h[:], in_=SC.h[:, 0:n], axis=AX.X, op=ALU.max,
                                                    apply_absolute_value=True), [SC], [A])
                op("dve", lambda e: e.tensor_scalar(out=u2.h[:], in0=A.h[:], scalar1=1.0, scalar2=2.0,
                                                    op0=ALU.add, op1=ALU.mult), [A], [u2])
                op("dve", lambda e: e.tensor_scalar(out=lo.h[:], in0=u2.h[:], scalar1=-0.5, scalar2=None,
                                                    op0=ALU.mult), [u2], [lo])
                op("dve", lambda e: e.tensor_scalar(out=stp[ch].h[:], in0=pw.h[:], scalar1=u2.h[:], scalar2=None,
                                                    op0=ALU.mult), [pw, u2], [stp[ch]])
                op("dve", lambda e: e.memset(mid.h[:], 0.0), [], [mid])
                if ch == 1:
                    op("dve", lambda e: e.tensor_scalar(out=nstp[ch].h[:], in0=stp[ch].h[:], scalar1=-1.0, scalar2=None,
                                                        op0=ALU.mult), [stp[ch]], [nstp[ch]])
                    op("dve", lambda e: e.memset(tq.h[:], 0.0), [], [tq])
                op("pool", lambda e: e.tensor_tensor(out=SC.h[:, n - 128:n], in0=SC.h[:, n - 128:n],
                                                     in1=cmtok.h[:], op=ALU.add), [SC, cmtok], [SC])

            def iter_dve(i, SC, it):
                n = 128 * (i + 1)
                A, lo, mid, cntt, ge, u2, tq, _ = sm[0]
                junk = junks[0]
                op("dve", lambda e: e.tensor_scalar(out=junk.h[:, 0:n], in0=SC.h[:, 0:n], scalar1=mid.h[:], scalar2=0.0,
                                                    op0=ALU.is_ge, op1=ALU.add, accum_out=cntt.h[:]),
                   [SC, mid], [junk, cntt])
                op("dve", lambda e: e.tensor_scalar(out=ge.h[:], in0=cntt.h[:], scalar1=float(TOPK) - 0.5, scalar2=0.5,
                                                    op0=ALU.is_ge, op1=ALU.subtract), [cntt], [ge])
                op("dve", lambda e: e.scalar_tensor_tensor(out=mid.h[:], in0=ge.h[:], scalar=stp[0].h[:, it:it + 1],
                                                           in1=mid.h[:], op0=ALU.mult, op1=ALU.add),
                   [ge, stp[0], mid], [mid])
                if it == NIT - 1:
                    op("dve", lambda e: e.tensor_tensor(out=lo.h[:], in0=mid.h[:], in1=stp[0].h[:, NIT:NIT + 1],
                                                        op=ALU.subtract), [mid, stp[0]], [lo])

            def iter_act(i, SC, it):
                n = 128 * (i + 1)
                A, lo, nmA, cs, sg, u2, nmB, _ = sm[1]
                nm_cur, nm_nxt = (nmB, nmA) if it % 2 == 0 else (nmA, nmB)
                junk = junks[1]
                op("act", lambda e: e.activation(out=junk.h[:, 0:n], in_=SC.h[:, 0:n], func=AF.Sign, bias=nm_cur.h[:],
                                                 accum_out=cs.h[:]), [SC, nm_cur], [junk, cs])
                op("act", lambda e: e.activation(out=sg.h[:], in_=cs.h[:], func=AF.Sign,
                                                 bias=float(n - 2 * TOPK) + 0.5), [cs], [sg])
                op("act", lambda e: e.activation(out=nm_nxt.h[:], in_=sg.h[:], func=AF.Identity,
                                                 scale=nstp[1].h[:, it + 1:it + 2], bias=nm_cur.h[:]),
                   [sg, nstp[1], nm_cur], [nm_nxt])
                if it == NIT - 1:
                    op("act", lambda e: e.activation(out=lo.h[:], in_=nm_nxt.h[:], func=AF.Identity, scale=-1.0,
                                                     bias=nstp[1].h[:, NIT:NIT + 1]), [nm_nxt, nstp[1]], [lo])

            def finish(i, SC, ch):
                n = 128 * (i + 1)
                q0 = 128 * i
                lo = sm[ch][1]
                mn = MNs[ch]
                op("dve", lambda e: e.tensor_scalar(out=mn.h[:, 0:n], in0=SC.h[:, 0:n], scalar1=lo.h[:], scalar2=None,
                                                    op0=ALU.is_ge), [SC, lo], [mn])
                for j0 in range(0, i + 1, 8):
                    nj = min(8, i + 1 - j0)
                    tp = TPm[cn["tp"] % 2]
                    cn["tp"] += 1
                    ts_ = tst[cn["ti"] % 3]
                    cn["ti"] += 1
                    for jj in range(nj):
                        j = j0 + jj
                        op("pe", lambda e, jj=jj, j=j: e.transpose(out=tp.h[:, jj, :], in_=mn.h[:, 128 * j:128 * j + 128],
                                                                   identity=identb.h[:]), [mn, identb], [tp])
                    op("act", lambda e: e.copy(out=ts_.h[:, 0:nj, :], in_=tp.h[:, 0:nj, :]), [tp], [ts_])
                    dma("sp", mnegd[128 * j0:128 * (j0 + nj), q0:q0 + 128].rearrange("(j p) t -> p j t", p=128),
                        ts_.h[:, 0:nj, :], reads=[ts_])

            for i0 in range(0, NB, 2):
                ia, ib = i0, i0 + 1
                scores(ib, SCs[1])
                scores(ia, SCs[0])
                prep(ib, SCs[1], None, 1)
                prep(ia, SCs[0], None, 0)
                for it in range(NIT):
                    if 128 * (ib + 1) > TOPK:
                        iter_act(ib, SCs[1], it)
                    if 128 * (ia + 1) > TOPK:
                        iter_dve(ia, SCs[0], it)
                finish(ib, SCs[1], 1)
                finish(ia, SCs[0], 0)
            sch.barrier()
            sch.emit()

    def phase_attn(maps, Kd, G, final, use_mneg=False, nunits=1, t5=False, prefetch=True):
        groups_ = [maps[g0:g0 + G] for g0 in range(0, len(maps), G)]
        nu = max(len(set(u for m in grp for u in m["vu"])) for grp in groups_)
        NKV = 2 if prefetch else 1
        with ExitStack() as es:
            Kts = [T(es, [128, G, S], BF16) for _ in range(NKV)]
            Vts = [T(es, [128, NB, nu, 65], BF16) for _ in range(NKV)]

            def load_kv(gidx):
                grp = groups_[gidx]
                Kt, Vt = Kts[gidx % NKV], Vts[gidx % NKV]
                units = sorted(set(u for m in grp for u in m["vu"]))
                for gi, m in enumerate(grp):
                    dma("sp", Kt.h[:Kd, gi, :], ka[m["ki"], 0:Kd, :], writes=[Kt], partial=True)
                for ui, u in enumerate(units):
                    for n0 in range(0, NB, 8):
                        n1 = min(NB, n0 + 8)
                        dma("sp", Vt.h[:, n0:n1, ui, :],
                            vv[128 * n0:128 * n1, u, :].rearrange("(n p) d -> p n d", p=128),
                            writes=[Vt], partial=True)

            Qt = [T(es, [128, G, 512], BF16) for _ in range(2)]
            NET = 4
            Et = [T(es, [128, 1024], BF16) for _ in range(NET)]
            NSP = 3
            Sp = [PS(es, [128, 1024]) for _ in range(NSP)]
            Op = [PS(es, [128, 512]) for _ in range(nunits)]
            if nunits == 1:
                Tp = PS(es, [128, 4, 128])
            else:
                Tp = TB(Sp[NSP - 1].h[:, 0:512].rearrange("p (u d) -> p u d", d=128))
                Tp.res = Sp[NSP - 1].res
            oT = [T(es, [65, 512], F32) for _ in range(2 * nunits)]
            if use_mneg:
                Mt = [T(es, [128, 8, 512], BF16) for _ in range(NB // 8)]
            fst = final["alloc"](es)
            cnts = {"s": 0, "e": 0, "o": 0}
            strips = None
            if t5:
                strips = T(es, [128, 8, 640], BF16)
                tmpa = T(es, [128, 640], F32)
                tmpb = T(es, [128, 640], F32)
                dma("sp", tmpa.h[:], I["c_cmstrip"], writes=[tmpa])
                for h in range(8):
                    dma("sp", tmpb.h[:], I["t5strip"][h], writes=[tmpb])
                    op("dve", lambda e, h=h: e.tensor_tensor(out=strips.h[:, h, :], in0=tmpb.h[:], in1=tmpa.h[:],
                                                            op=ALU.add), [tmpb, tmpa], [strips], partial=True)
            if prefetch:
                load_kv(0)
            for gidx, grp in enumerate(groups_):
                Kt, Vt = Kts[gidx % NKV], Vts[gidx % NKV]
                units = sorted(set(u for m in grp for u in m["vu"]))
                if prefetch:
                    if gidx + 1 < len(groups_):
                        load_kv(gidx + 1)
                else:
                    load_kv(gidx)
                for c in range(NCH):
                    qt = Qt[c % 2]
                    for gi, m in enumerate(grp):
                        dma("sp", qt.h[:Kd, gi, :], qa[m["qi"], 0:Kd, 512 * c:512 * c + 512], writes=[qt],
                            partial=True)
                    if use_mneg:
                        nkb = 4 * c + 4
                        for p0 in range(0, nkb, 8):
                            npz = min(8, nkb - p0)
                            dma("sp", Mt[p0 // 8].h[:, 0:npz, :],
                                mnegd[128 * p0:128 * (p0 + npz), 512 * c:512 * c + 512].rearrange("(j p) t -> p j t", p=128),
                                writes=[Mt[p0 // 8]])
                    for gi, m in enumerate(grp):
                        ui_list = [units.index(u) for u in m["vu"]]
                        groups = []
                        if m["strip"] == "c":
                            sap = cstrip.h[:, :]
                            sres = cstrip
                        else:
                            sap = strips.h[:, m["strip"], :]
                            sres = strips
                        nfar = max(4 * c - 1, 0) if m["near"] else 4 * c
                        j = 0
                        while j < nfar:
                            if j + 1 < nfar:
                                groups.append([(j, 0, None), (j + 1, 0, None)])
                                j += 2
                            else:
                                groups.append([(j, 0, None)])
                                j += 1
                        for j in range(nfar, 4 * c + 4):
                            r = j - 4 * c
                            if r < 0:
                                groups.append([(j, 0, 128)])
                            else:
                                groups.append([(j, 128 * r, 0)])
                        last_j = 4 * c + 3

                        def emit_qk(grpb):
                            sp = Sp[cnts["s"] % NSP]
                            cnts["s"] += 1
                            for bi, (j, col0, off) in enumerate(grpb):
                                base = 512 * bi
                                N = 512 - col0
                                extra = (off is not None)
                                op("pe", lambda e, sp=sp, j=j, col0=col0, N=N, base=base, extra=extra: e.matmul(
                                    sp.h[:, base + col0:base + 512], Kt.h[:Kd, gi, 128 * j:128 * j + 128],
                                    qt.h[:Kd, gi, col0:512], start=True, stop=not extra), [Kt, qt], [sp])
                                if off is not None:
                                    NN = N if m["near"] else 128
                                    op("pe", lambda e, sp=sp, col0=col0, NN=NN, base=base, off=off: e.matmul(
                                        sp.h[:, base + col0:base + col0 + NN], identb.h[:], sap[:, off:off + NN],
                                        start=False, stop=True), [identb, sres], [sp])
                            return sp

                        def emit_exp(grpb, sp):
                            et = Et[cnts["e"] % NET]
                            cnts["e"] += 1
                            col0 = grpb[0][1]
                            far = grpb[0][2] is None
                            bias = m["bias"] if (far and m["bias"] is not None) else None
                            if len(grpb) == 2:
                                src, dst = sp.h[:, 0:1024], et.h[:, 0:1024]
                            else:
                                src, dst = sp.h[:, col0:512], et.h[:, col0:512]
                            if bias is not None:
                                op("act", lambda e, src=src, dst=dst, bias=bias: e.activation(
                                    out=dst, in_=src, func=AF.Exp, bias=bias), [sp, b31], [et])
                            else:
                                op("act", lambda e, src=src, dst=dst: e.activation(out=dst, in_=src, func=AF.Exp),
                                   [sp], [et])
                            if use_mneg:
                                j = grpb[0][0]
                                mt = Mt[j // 8]
                                if len(grpb) == 2:
                                    msk = mt.h[:, j % 8:j % 8 + 2, :].rearrange("p j t -> p (j t)")
                                else:
                                    msk = mt.h[:, j % 8, col0:512]
                                meng = "dve"
                                op(meng, lambda e, dst=dst, msk=msk: e.tensor_tensor(out=dst, in0=dst, in1=msk,
                                                                                      op=ALU.mult), [et, mt], [et])
                            return et

                        def emit_pv(grpb, et):
                            for bi, (j, col0, off) in enumerate(grpb):
                                base = 512 * bi
                                for k, ui in enumerate(ui_list):
                                    op("pe", lambda e, j=j, col0=col0, base=base, k=k, ui=ui: e.matmul(
                                        Op[k].h[0:65, col0:512], Vt.h[:, j, ui, :], et.h[:, base + col0:base + 512],
                                        start=(j == 0), stop=(j == last_j)), [Vt, et], [Op[k]])

                        sps = [None] * len(groups)
                        LA = NSP - 1
                        PVLAG = 1
                        pend_pv = []
                        for t in range(min(LA, len(groups))):
                            sps[t] = emit_qk(groups[t])
                        for t in range(len(groups)):
                            if t + LA < len(groups):
                                sps[t + LA] = emit_qk(groups[t + LA])
                            et = emit_exp(groups[t], sps[t])
                            pend_pv.append((groups[t], et))
                            if len(pend_pv) > PVLAG:
                                emit_pv(*pend_pv.pop(0))
                        while pend_pv:
                            emit_pv(*pend_pv.pop(0))
                        tps = []
                        for k in range(len(ui_list)):
                            o = oT[cnts["o"] % len(oT)]
                            cnts["o"] += 1
                            op("dve", lambda e, o=o, k=k: e.tensor_copy(out=o.h[:, :], in_=Op[k].h[0:65, :]),
                               [Op[k]], [o])
                            tps.append(o)
                        final["fn"](fst, m, c, tps, Tp)
                if not prefetch:
                    sch.barrier()
                sch.emit()
            sch.barrier()
            sch.emit()

    def std_final():
        def alloc(es):
            return {"rec": [T(es, [128, 4, 1], F32) for _ in range(2)],
                    "stg": [T(es, [128, 4, 64], BF16) for _ in range(2)], "i": 0}

        def fn(st, m, c, tps, Tp):
            i = st["i"]
            st["i"] += 1
            rec, stg = st["rec"][i % 2], st["stg"][i % 2]
            o = tps[0]
            for u in range(4):
                op("pe", lambda e, u=u: e.transpose(out=Tp.h[:, u, 0:65], in_=o.h[0:65, 128 * u:128 * u + 128],
                                                    identity=identf.h[0:65, 0:65]), [o, identf], [Tp])
            op("dve", lambda e: e.reciprocal(out=rec.h[:], in_=Tp.h[:, :, 64:65]), [Tp], [rec])
            op("dve", lambda e: e.tensor_tensor(out=stg.h[:], in0=Tp.h[:, :, 0:64],
                                                in1=rec.h[:].to_broadcast([128, 4, 64]), op=ALU.mult),
               [Tp, rec], [stg])
            col = m["col"]
            dma("pool", mixd[512 * c:512 * c + 512, col:col + 64].rearrange("(u p) d -> p u d", p=128), stg.h[:],
                reads=[stg])
        return {"alloc": alloc, "fn": fn}

    def phase_proj_odd(xsrc):
        QS = 96.0 ** -0.5
        with ExitStack() as es:
            g = load_gain(es, I["norm_mix_g"][1], D)
            gq = load_gain(es, I["mla_q_norm_g"][0], 256)
            gkv = load_gain(es, I["mla_kv_norm_g"][0], 128)
            W = T(es, [128, 8, 1984], BF16)
            Wuq = T(es, [128, 2, 1024], BF16)
            Wukv = T(es, [128, 1, 1024], BF16)
            stg = [T(es, [128, 512], F32) for _ in range(3)]
            src = I["w_in_odd"]
            for k in range(8):
                r0 = 128 * k
                load_w(stg, W, k, 0, src, r0, 0, 512, gs=g, gk=k, mul=0.125)
                load_w(stg, W, k, 512, src, r0, 512, 512, gs=g, gk=k)
                load_w(stg, W, k, 1024, src, r0, 1920, 32, gs=g, gk=k)
                load_w(stg, W, k, 1056, src, r0, 1936, 16, gs=g, gk=k, mul=-1.0)
                load_w(stg, W, k, 1072, src, r0, 1920, 16, gs=g, gk=k)
                load_w(stg, W, k, 1088, src, r0, 1024, 512, gs=g, gk=k)
                load_w(stg, W, k, 1600, src, r0, 1536, 384, gs=g, gk=k)
            for k in range(2):
                r0 = 128 * k
                for h in range(8):
                    load_w(stg, Wuq, k, 64 * h, I["w_mla_uq"], r0, 96 * h, 64, gs=gq, gk=k, mul=QS)
                    load_w(stg, Wuq, k, 512 + 32 * h, I["w_mla_uq"], r0, 96 * h + 64, 32, gs=gq, gk=k, mul=QS)
                    load_w(stg, Wuq, k, 768 + 32 * h, I["w_mla_uq"], r0, 96 * h + 80, 16, gs=gq, gk=k, mul=-QS)
                    load_w(stg, Wuq, k, 768 + 32 * h + 16, I["w_mla_uq"], r0, 96 * h + 64, 16, gs=gq, gk=k, mul=QS)
            for h in range(8):
                load_w(stg, Wukv, 0, 64 * h, I["w_mla_ukv"], 0, 128 * h, 64, gs=gkv, gk=0)
                load_w(stg, Wukv, 0, 512 + 64 * h, I["w_mla_ukv"], 0, 128 * h + 64, 64, gs=gkv, gk=0)
            nst = make_norm_T(es, 8)
            hTs = [T(es, [128, 8, 512], BF16) for _ in range(2)]
            mcT = T(es, [128, 3, 512], BF16)
            pra = PS(es, [128, 512])
            prb = PS(es, [128, 512])
            pfm = [pra, prb]
            ptm = [PS(es, [128, 512]) for _ in range(2)]
            tpm = PS(es, [128, 3, 128], BF16)
            fst = [T(es, [128, 512], BF16) for _ in range(7)]
            vst = [T(es, [128, 16, 65], BF16) for _ in range(2)]
            for v in vst:
                op("pool", lambda e, v=v: e.memset(v.h[:], 1.0), [], [v])
            zerob = T(es, [8, 512], BF16)
            op("pool", lambda e: e.memset(zerob.h[:], 0.0), [], [zerob])
            for h in range(8):
                for c in range(NCH):
                    dma("pool", qa[h, 64:72, 512 * c:512 * c + 512], zerob.h[:, :], reads=[zerob])
                    dma("pool", ka[h, 64:72, 512 * c:512 * c + 512], zerob.h[:, :], reads=[zerob])
            cs4 = [T(es, [128, 512], F32) for _ in range(2)]
            sn4 = [T(es, [128, 512], F32) for _ in range(2)]
            t1 = [T(es, [128, 512], F32) for _ in range(2)]
            t2 = [T(es, [128, 512], F32) for _ in range(2)]
            sq = [T(es, [128, 1], F32) for _ in range(4)]
            rs = [T(es, [128, 1], F32) for _ in range(4)]
            xn = [T(es, [128, 384], BF16) for _ in range(2)]
            junk = nst["junk"]
            gi = 0
            ri = 0

            def evac(ps, st, M):
                nonlocal gi
                gi += 1
                if gi % 2 == 0:
                    op("act", lambda e: e.copy(out=st.h[:M, :], in_=ps.h[:M, :]), [ps], [st])
                else:
                    op("dve", lambda e: e.tensor_copy(out=st.h[:M, :], in_=ps.h[:M, :]), [ps], [st])

            def rope(pa, pb, M, cst, snt, st):
                nonlocal ri
                a, b = t1[ri % 2], t2[ri % 2]
                ri += 1
                op("dve", lambda e: e.tensor_tensor(out=a.h[:M, :], in0=pa.h[:M, :], in1=cst.h[:M, :], op=ALU.mult),
                   [pa, cst], [a])
                op("dve", lambda e: e.tensor_tensor(out=b.h[:M, :], in0=pb.h[:M, :], in1=snt.h[:M, :], op=ALU.mult),
                   [pb, snt], [b])
                op("pool", lambda e: e.tensor_tensor(out=st.h[:M, :], in0=a.h[:M, :], in1=b.h[:M, :], op=ALU.add),
                   [a, b], [st])

            def load_x(c):
                for b in range(4):
                    blk = 4 * c + b
                    xt = nst["xt"][blk % 8]
                    dma("sp", xt.h[:], xsrc[128 * blk:128 * blk + 128, :], writes=[xt])

            for c in range(NCH):
                hT = hTs[c % 2]
                cs = slice(512 * c, 512 * c + 512)
                cst, snt = cs4[c % 2], sn4[c % 2]
                dma("act", cst.h[:], I["c_cos4"][:, cs], writes=[cst])
                dma("act", snt.h[:], I["c_sin4"][:, cs], writes=[snt])
                if c == 0:
                    load_x(0)
                if c + 1 < NCH:
                    load_x(c + 1)
                for b in range(4):
                    blk = 4 * c + b
                    xt = nst["xt"][blk % 8]
                    norm_T(nst, xt, hT, b)
                for gidx in range(8):
                    ps, st = pfm[gidx % 2], fst[gidx % 4]
                    fm_group(W, 8, 128 * gidx, 128, hT, ps)
                    evac(ps, st, 128)
                    for half in range(2):
                        hd = 2 * gidx + half
                        dst = qa[hd, 0:64, cs] if hd < 8 else ka[hd - 8, 0:64, cs]
                        dma("sp", dst, st.h[64 * half:64 * half + 64, :], reads=[st])
                fm_group(W, 8, 1024, 32, hT, pra)
                fm_group(W, 8, 1056, 32, hT, prb)
                st = fst[6]
                rope(pra, prb, 32, cst, snt, st)
                for h in range(8):
                    dma("pool", ka[8 + h, 64:96, cs], st.h[0:32, :], reads=[st])
                for b in range(4):
                    blk = 4 * c + b
                    tok = slice(128 * blk, 128 * blk + 128)
                    vs = vst[blk % 2]
                    ps = ptm[0]
                    for k in range(8):
                        op("pe", lambda e, k=k: e.matmul(ps.h[:, :], hT.h[:, k, 128 * b:128 * b + 128],
                                                         W.h[:, k, 1088:1600], start=(k == 0), stop=(k == 7)),
                           [W, hT], [ps])
                    op("act", lambda e: e.copy(out=vs.h[:, 0:8, 0:64],
                                               in_=ps.h[:, :].rearrange("p (h d) -> p h d", d=64)), [ps], [vs],
                       partial=True)
                    ps = ptm[1]
                    for k in range(8):
                        op("pe", lambda e, k=k: e.matmul(ps.h[:, 0:384], hT.h[:, k, 128 * b:128 * b + 128],
                                                         W.h[:, k, 1600:1984], start=(k == 0), stop=(k == 7)),
                           [W, hT], [ps])
                    x_ = xn[blk % 2]
                    for pi, (a0, a1) in enumerate(((0, 256), (256, 384))):
                        ssq, rstd = sq[2 * (blk % 2) + pi], rs[2 * (blk % 2) + pi]
                        op("act", lambda e: e.activation(out=junk.h[:, a0:a1], in_=ps.h[:, a0:a1], func=AF.Square,
                                                         accum_out=ssq.h[:]), [ps], [junk, ssq])
                        rstd_of(ssq, rstd, a1 - a0)
                        op("dve", lambda e: e.tensor_scalar(out=x_.h[:, a0:a1], in0=ps.h[:, a0:a1], scalar1=rstd.h[:],
                                                            scalar2=None, op0=ALU.mult), [ps, rstd], [x_], partial=True)
                    for k in range(3):
                        op("pe", lambda e, k=k: e.transpose(out=tpm.h[:, k, :], in_=x_.h[:, 128 * k:128 * k + 128],
                                                            identity=identb.h[:]), [x_, identb], [tpm])
                    op("act", lambda e: e.copy(out=mcT.h[:, :, 128 * b:128 * b + 128], in_=tpm.h[:]), [tpm], [mcT],
                       partial=True)
                    ps = ptm[0]
                    op("pe", lambda e: e.matmul(ps.h[:, :], mcT.h[:, 2, 128 * b:128 * b + 128], Wukv.h[:, 0, 512:1024],
                                                start=True, stop=True), [mcT, Wukv], [ps])
                    op("dve", lambda e: e.tensor_copy(out=vs.h[:, 8:16, 0:64],
                                                      in_=ps.h[:, :].rearrange("p (h d) -> p h d", d=64)), [ps], [vs],
                       partial=True)
                    dma("pool", vv[tok, :, :], vs.h[:], reads=[vs])
                for gidx in range(4):
                    ps, st = pfm[gidx % 2], fst[4 + gidx % 2]
                    fm_group(Wuq, 2, 128 * gidx, 128, mcT, ps)
                    evac(ps, st, 128)
                    for half in range(2):
                        dma("sp", qa[8 + 2 * gidx + half, 0:64, cs], st.h[64 * half:64 * half + 64, :], reads=[st])
                for grp in range(2):
                    fm_group(Wuq, 2, 512 + 128 * grp, 128, mcT, pra)
                    fm_group(Wuq, 2, 768 + 128 * grp, 128, mcT, prb)
                    st = fst[6]
                    rope(pra, prb, 128, cst, snt, st)
                    for i in range(4):
                        dma("pool", qa[8 + 4 * grp + i, 64:96, cs], st.h[32 * i:32 * i + 32, :], reads=[st])
                for gidx in range(4):
                    ps, st = pfm[gidx % 2], fst[4 + gidx % 2]
                    fm_group(Wukv, 1, 128 * gidx, 128, mcT, ps, kbase=2)
                    evac(ps, st, 128)
                    for half in range(2):
                        dma("sp", ka[8 + 2 * gidx + half, 0:64, cs], st.h[64 * half:64 * half + 64, :], reads=[st])
            sch.barrier()
            sch.emit()

    def diff_final(lam_init):
        def alloc(es):
            st = {"rec": [T(es, [128, 4, 1], F32) for _ in range(2)],
                  "on": [T(es, [128, 4, 128], F32) for _ in range(2)],
                  "a": T(es, [128, 4, 128], F32), "junk": T(es, [128, 128], BF16),
                  "ssq": T(es, [128, 4], F32), "rstd": T(es, [128, 4], F32),
                  "stg": [T(es, [128, 4, 128], BF16) for _ in range(2)], "i": 0,
                  "nl": T(es, [128, 1], F32)}
            lv = [T(es, [128, 64], F32) for _ in range(4)]
            for t, nm in zip(lv, ("lambda_q1", "lambda_k1", "lambda_q2", "lambda_k2")):
                dma("sp", t.h[:], I[nm].partition_broadcast(128), writes=[t])
            pr = T(es, [128, 64], F32)
            e1 = T(es, [128, 1], F32)
            e2 = T(es, [128, 1], F32)
            for (a, b, d) in ((lv[0], lv[1], e1), (lv[2], lv[3], e2)):
                op("dve", lambda e: e.tensor_tensor(out=pr.h[:], in0=a.h[:], in1=b.h[:], op=ALU.mult), [a, b], [pr])
                op("dve", lambda e: e.reduce_sum(out=d.h[:], in_=pr.h[:], axis=AX.X), [pr], [d])
                op("act", lambda e: e.activation(out=d.h[:], in_=d.h[:], func=AF.Exp), [d], [d])
            nl = st["nl"]
            op("dve", lambda e: e.scalar_tensor_tensor(out=nl.h[:], in0=e2.h[:], scalar=-float(lam_init), in1=e1.h[:],
                                                       op0=ALU.add, op1=ALU.subtract), [e1, e2], [nl])
            return st

        def fn(st, m, c, tps, Tp):
            j = m["qi"] % 2
            h = m["qi"] // 2
            on = st["on"][j]
            for k, o in enumerate(tps):
                rec = st["rec"][k]
                for u in range(4):
                    op("pe", lambda e, u=u: e.transpose(out=Tp.h[:, u, 0:65], in_=o.h[0:65, 128 * u:128 * u + 128],
                                                        identity=identf.h[0:65, 0:65]), [o, identf], [Tp])
                op("dve", lambda e: e.reciprocal(out=rec.h[:], in_=Tp.h[:, :, 64:65]), [Tp], [rec])
                op("dve", lambda e: e.tensor_tensor(out=on.h[:, :, 64 * k:64 * k + 64], in0=Tp.h[:, :, 0:64],
                                                    in1=rec.h[:].to_broadcast([128, 4, 64]), op=ALU.mult),
                   [Tp, rec], [on], partial=True)
            if j == 0:
                return
            i = st["i"]
            st["i"] += 1
            a, stg, ssq, rstd, junk, nl = st["a"], st["stg"][i % 2], st["ssq"], st["rstd"], st["junk"], st["nl"]
            on0, on1 = st["on"]
            op("dve", lambda e: e.scalar_tensor_tensor(out=a.h[:], in0=on1.h[:], scalar=nl.h[:], in1=on0.h[:],
                                                       op0=ALU.mult, op1=ALU.add), [on0, on1, nl], [a])
            for u in range(4):
                op("act", lambda e, u=u: e.activation(out=junk.h[:], in_=a.h[:, u, :], func=AF.Square,
                                                      accum_out=ssq.h[:, u:u + 1]), [a], [junk, ssq], partial=True)
            op("act", lambda e: e.activation(out=rstd.h[:], in_=ssq.h[:], func=AF.Sqrt, bias=epsb.h[:], scale=1.0 / 128),
               [ssq, epsb], [rstd])
            op("dve", lambda e: e.reciprocal(out=rstd.h[:], in_=rstd.h[:]), [rstd], [rstd])
            op("dve", lambda e: e.tensor_tensor(out=stg.h[:], in0=a.h[:],
                                                in1=rstd.h[:].unsqueeze(2).to_broadcast([128, 4, 128]), op=ALU.mult),
               [a, rstd], [stg])
            dma("pool", mixd[512 * c:512 * c + 512, 128 * h:128 * h + 128].rearrange("(u p) d -> p u d", p=128),
                stg.h[:], reads=[stg])
        return {"alloc": alloc, "fn": fn}

    def phase_outproj(w_src, xsrc, xdst, rowscale=None):
        with ExitStack() as es:
            Wo = T(es, [128, 8, D], BF16)
            stg = [T(es, [128, 1024], F32) for _ in range(3)]
            for k in range(8):
                if rowscale is not None and rowscale[k] is not None:
                    load_w(stg, Wo, k, 0, w_src, 128 * k, 0, D, gs=rowscale[k][0], gk=0, mul=rowscale[k][1])
                else:
                    load_w(stg, Wo, k, 0, w_src, 128 * k, 0, D)
            mx = [T(es, [128, D], BF16) for _ in range(3)]
            xt = [T(es, [128, D], F32) for _ in range(3)]
            xo = [T(es, [128, D], F32) for _ in range(2)]
            tp = [PS(es, [128, 8, 128], BF16) for _ in range(2)]
            mT = [T(es, [128, 8, 128], BF16) for _ in range(2)]
            po = [PS(es, [128, 1024]) for _ in range(2)]
            for blk in range(NB):
                tok = slice(128 * blk, 128 * blk + 128)
                m_, x_, o_, t_, mt_, p_ = mx[blk % 3], xt[blk % 3], xo[blk % 2], tp[blk % 2], mT[blk % 2], po[blk % 2]
                dma("sp", m_.h[:], mixd[tok, :], writes=[m_])
                dma("act", x_.h[:], xsrc[tok, :], writes=[x_])
                for k in range(8):
                    op("pe", lambda e, k=k, t_=t_, m_=m_: e.transpose(out=t_.h[:, k, :], in_=m_.h[:, 128 * k:128 * k + 128],
                                                                      identity=identb.h[:]), [m_, identb], [t_])
                op("act", lambda e, mt_=mt_, t_=t_: e.copy(out=mt_.h[:], in_=t_.h[:]), [t_], [mt_])
                for hf in range(2):
                    for k in range(8):
                        op("pe", lambda e, k=k, hf=hf, p_=p_, mt_=mt_: e.matmul(
                            p_.h[:, 512 * hf:512 * hf + 512], mt_.h[:, k, :], Wo.h[:, k, 512 * hf:512 * hf + 512],
                            start=(k == 0), stop=(k == 7)), [mt_, Wo], [p_])
                op("dve", lambda e, o_=o_, p_=p_, x_=x_: e.tensor_tensor(out=o_.h[:], in0=p_.h[:], in1=x_.h[:], op=ALU.add),
                   [p_, x_], [o_])
                dma("pool", xdst[tok, :], o_.h[:], reads=[o_])
            sch.barrier()
            sch.emit()

    def phase_ffn(layer, xsrc, xdst, final_g=None):
        with ExitStack() as es:

            g = load_gain(es, I["norm_ffn_g"][layer], D)
            Wg = T(es, [128, 8, FFN], BF16)
            Wu = T(es, [128, 8, FFN], BF16)
            Wd = T(es, [128, 22, D], BF16)
            stg = [T(es, [128, 704], F32) for _ in range(2)]
            gres = [Res() for _ in range(4)]
            ures = [Res() for _ in range(4)]
            for cc in range(4):
                for k in range(8):
                    load_w(stg, Wg, k, 704 * cc, I["w_ffn_gate"][layer], 128 * k, 704 * cc, 704, gs=g, gk=k, res=gres[cc])
                    load_w(stg, Wu, k, 704 * cc, I["w_ffn_up"][layer], 128 * k, 704 * cc, 704, gs=g, gk=k, res=ures[cc])
            for f in range(22):
                load_w(stg, Wd, f, 0, I["w_ffn_down"][layer], 128 * f, 0, D)
            nst = make_norm_T(es)
            hT = T(es, [128, 8, 512], BF16)
            zT = T(es, [128, 22, 512], BF16)
            sg = [T(es, [128, 512], BF16) for _ in range(2)]
            pg = [PS(es, [128, 512]) for _ in range(2)]
            pu = [PS(es, [128, 512]) for _ in range(2)]
            pd = [PS(es, [128, 1024]) for _ in range(1)]
            xr = [T(es, [128, D], F32) for _ in range(2)]
            if final_g is not None:
                gf = T(es, [128, D], F32)
                dma("sp", gf.h[:], final_g.partition_broadcast(128), writes=[gf])
                fs = [T(es, [128, 1], F32) for _ in range(4)]
                fj = nst["junk"]
            for c in range(NCH):
                for b in range(4):
                    blk = 4 * c + b
                    xt = nst["xt"][blk % 3]
                    dma("sp", xt.h[:], xsrc[128 * blk:128 * blk + 128, :], writes=[xt])
                    norm_T(nst, xt, hT, b)
                for f in range(22):
                    g_, u_, s_ = pg[f % 2], pu[f % 2], sg[f % 2]
                    c_lo, c_hi = (128 * f) // 704, (128 * f + 127) // 704
                    if c_lo == c_hi:
                        fm_group(Wg, 8, 128 * f, 128, hT, g_, wres=gres[c_lo])
                        fm_group(Wu, 8, 128 * f, 128, hT, u_, wres=ures[c_lo])
                    else:
                        op("pe", lambda e: e.matmul(g_.h[:, :], Wg.h[:, 0, 128 * f:128 * f + 128], hT.h[:, 0, :],
                                                    start=True, stop=False), [gres[c_lo], gres[c_hi], hT], [g_])
                        for k in range(1, 8):
                            op("pe", lambda e, k=k: e.matmul(g_.h[:, :], Wg.h[:, k, 128 * f:128 * f + 128], hT.h[:, k, :],
                                                             start=False, stop=(k == 7)), [gres[c_lo], hT], [g_])
                        op("pe", lambda e: e.matmul(u_.h[:, :], Wu.h[:, 0, 128 * f:128 * f + 128], hT.h[:, 0, :],
                                                    start=True, stop=False), [ures[c_lo], ures[c_hi], hT], [u_])
                        for k in range(1, 8):
                            op("pe", lambda e, k=k: e.matmul(u_.h[:, :], Wu.h[:, k, 128 * f:128 * f + 128], hT.h[:, k, :],
                                                             start=False, stop=(k == 7)), [ures[c_lo], hT], [u_])
                    op("act", lambda e, s_=s_, g_=g_: e.activation(out=s_.h[:], in_=g_.h[:], func=AF.Silu), [g_], [s_])
                    op("dve", lambda e, f=f, s_=s_, u_=u_: e.tensor_tensor(out=zT.h[:, f, :], in0=u_.h[:], in1=s_.h[:],
                                                                         op=ALU.mult), [u_, s_], [zT], partial=True)
                for b in range(4):
                    blk = 4 * c + b
                    p_ = pd[0]
                    o_ = xr[blk % 2]
                    dma("act", o_.h[:], xsrc[128 * blk:128 * blk + 128, :], writes=[o_])
                    for hf in range(2):
                        for f in range(22):
                            op("pe", lambda e, f=f, hf=hf, b=b, p_=p_: e.matmul(
                                p_.h[:, 512 * hf:512 * hf + 512], zT.h[:, f, 128 * b:128 * b + 128],
                                Wd.h[:, f, 512 * hf:512 * hf + 512], start=(f == 0), stop=(f == 21)), [zT, Wd], [p_])
                    op("dve", lambda e, o_=o_, p_=p_: e.tensor_tensor(out=o_.h[:], in0=p_.h[:], in1=o_.h[:],
                                                                      op=ALU.add), [p_, o_], [o_])
                    if final_g is not None:
                        ssq, rstd = fs[2 * (blk % 2)], fs[2 * (blk % 2) + 1]
                        op("act", lambda e, o_=o_, ssq=ssq: e.activation(out=fj.h[:], in_=o_.h[:], func=AF.Square,
                                                                         accum_out=ssq.h[:]), [o_], [fj, ssq])
                        rstd_of(ssq, rstd, D)
                        op("dve", lambda e, o_=o_, rstd=rstd: e.scalar_tensor_tensor(
                            out=o_.h[:], in0=o_.h[:], scalar=rstd.h[:], in1=gf.h[:], op0=ALU.mult, op1=ALU.mult),
                           [o_, rstd, gf], [o_])
                    dma("pool", xdst[128 * blk:128 * blk + 128, :], o_.h[:], reads=[o_])
            sch.barrier()
            sch.emit()

    setup_consts()
    sf = std_final()
    if 0 in layers:
        phase_proj_even(I["x"])
        phase_index()
        fox_maps = [dict(qi=h, ki=h, vu=[h], strip="c", near=False, bias=None, col=64 * h) for h in range(8)]
        phase_attn(fox_maps, 70, 2, sf)
        dsa_maps = [dict(qi=8 + h, ki=8 + h, vu=[8 + h], strip=h, near=True, bias=b31.h[:, h:h + 1],
                         col=512 + 64 * h) for h in range(8)]
        phase_attn(dsa_maps, 72, 4, sf, use_mneg=True, t5=True, prefetch=False)
        phase_outproj(I["w_out_even"], I["x"], xa)
        phase_ffn(0, xa, xb if 1 in layers else y_out, final_g=None if 1 in layers else I["final_norm_g"][0])
    if 1 in layers:
        LI = 0.8 - 0.6 * math.exp(-0.3 * 1)
        xin1 = xb if 0 in layers else I["x"]
        phase_proj_odd(xin1)
        diff_maps = [dict(qi=m, ki=m, vu=[2 * (m // 2), 2 * (m // 2) + 1], strip=m, near=True, bias=b31.h[:, m:m + 1])
                     for m in range(8)]
        phase_attn(diff_maps, 72, 2, diff_final(LI), nunits=2, t5=True)
        mla_maps = [dict(qi=8 + h, ki=8 + h, vu=[8 + h], strip="c", near=False, bias=None, col=512 + 64 * h)
                    for h in range(8)]
        phase_attn(mla_maps, 96, 2, sf)
        with ExitStack() as es2:
            sg_ = T(es2, [128, 1], F32)
            dma("sp", sg_.h[:], I["diff_subln_g"].rearrange("o d -> d o"), writes=[sg_], allow_slow_non_contiguous=True)
            rsc = [(sg_, 1.0 - LI)] * 4 + [None] * 4
            phase_outproj(I["w_out_odd"], xin1, xa, rowscale=rsc)
        phase_ffn(1, xa, y_out, final_g=I["final_norm_g"][0])
    sch.barrier()
    top.close()
    return nc


_CACHE = {}


def _host_inputs(inputs, S, b):
    consts, bucket = _consts(S)
    t5 = np.asarray(inputs["t5_bias"], dtype=np.float32)
    m = {"x": np.ascontiguousarray(np.asarray(inputs["x"])[b])}
    for k in ("norm_mix_g", "norm_ffn_g", "w_ffn_gate", "w_ffn_up", "w_ffn_down"):
        m[k] = np.ascontiguousarray(np.asarray(inputs[k], dtype=np.float32))
    for k in ("w_in_even", "w_out_even", "w_in_odd", "w_mla_uq", "w_mla_ukv", "w_out_odd"):
        m[k] = np.ascontiguousarray(np.asarray(inputs[k], dtype=np.float32)[0])
    for k in ("b_forget", "lambda_q1", "lambda_k1", "lambda_q2", "lambda_k2", "diff_subln_g", "mla_q_norm_g",
              "mla_kv_norm_g"):
        m[k] = np.ascontiguousarray(np.asarray(inputs[k], dtype=np.float32).reshape(1, -1))
    m["final_norm_g"] = np.ascontiguousarray(np.asarray(inputs["final_norm_g"], dtype=np.float32).reshape(1, -1))
    m["t5_31"] = np.ascontiguousarray(t5[31:32, :])
    m["t5strip"] = np.ascontiguousarray(np.transpose(t5[bucket], (2, 0, 1)))
    m.update(consts)
    return m


def kernel(**inputs):
    x = np.asarray(inputs["x"])
    B, S, _ = x.shape
    key = (S,)
    if key not in _CACHE:
        _CACHE[key] = build(S)
    nc = _CACHE[key]
    in_maps = [_host_inputs(inputs, S, b) for b in range(B)]
    res = run_bass_kernel_spmd(nc, in_maps, core_ids=list(range(B)))
    return np.stack([np.asarray(r["y"], dtype=np.float32) for r in res.results], axis=0)
```
